# Optimizing a Trainium2 kernel written in Bass

```python
import math
import jax, jax.numpy as jnp
from jax import lax
import numpy as np

D_MODEL = 1024
BATCH = 2
SEQ = 8192
DEPTH = 1

CHUNK = 64
QBLOCK = 128
EPS = 1e-6
NEG_INF = -1e30

DA_HEADS = 4
DA_QK_DIM = 64
DA_V_DIM = 128
DA_WIDTH = DA_HEADS * DA_V_DIM

GDN_HEADS = 4
GDN_K_DIM = 128
GDN_V_DIM = 128
GDN_WIDTH = GDN_HEADS * GDN_V_DIM
CONV_K = 4
GDN_CONV_DIM = 2 * GDN_HEADS * GDN_K_DIM + GDN_HEADS * GDN_V_DIM

N_BRANCH = 2

N_GROUPS = 4
EXPERTS_PER_GROUP = 8
N_EXPERTS = N_GROUPS * EXPERTS_PER_GROUP
TOP_K = 2
D_EXPERT = 512
MOE_BLOCK = 128

IN_SPLITS = [DA_HEADS * 2 * DA_QK_DIM, DA_HEADS * 2 * DA_QK_DIM, DA_HEADS * DA_V_DIM,
             GDN_HEADS * GDN_K_DIM, GDN_HEADS * GDN_K_DIM, GDN_HEADS * GDN_V_DIM, GDN_HEADS * GDN_V_DIM,
             GDN_HEADS, GDN_HEADS, N_BRANCH * D_MODEL]
D_IN = sum(IN_SPLITS)

kernel_name = 'hybrid_diffattn_gdn_hmoe_adaln'


def rms_norm(x, gain):
    xf = x.astype(jnp.float32)
    y = xf * lax.rsqrt(jnp.mean(xf * xf, axis=-1, keepdims=True) + EPS)
    return (y * gain.astype(jnp.float32)).astype(x.dtype)


def l2_norm(x):
    xf = x.astype(jnp.float32)
    return xf * lax.rsqrt(jnp.sum(xf * xf, axis=-1, keepdims=True) + EPS)


def split_points(sizes):
    pts, acc = [], 0
    for s in sizes[:-1]:
        acc += s
        pts.append(acc)
    return pts


def causal_conv(u, w):
    return lax.conv_general_dilated(u, w[:, None, :].astype(u.dtype), (1,), [(CONV_K - 1, 0)],
                                    dimension_numbers=('NWC', 'WIO', 'NWC'),
                                    feature_group_count=u.shape[-1])


def diff_attention(q, k, v, lam):
    Bn, H, S = v.shape[:3]
    nb = S // QBLOCK
    k1, k2 = k[..., 0, :], k[..., 1, :]
    qb = q.reshape(Bn, H, nb, QBLOCK, 2, DA_QK_DIM).transpose(2, 0, 1, 3, 4, 5)
    slopes = 2.0 ** (-8.0 * jnp.arange(1, H + 1, dtype=jnp.float32) / H)
    kpos = jnp.arange(S)
    scale = DA_QK_DIM ** -0.5

    def block(args):
        i, qblk = args
        qpos = i * QBLOCK + jnp.arange(QBLOCK)
        allowed = (kpos[None, :] // CHUNK) <= (qpos[:, None] // CHUNK)
        dist = jnp.abs(qpos[:, None] - kpos[None, :]).astype(jnp.float32)
        bias = -slopes[:, None, None] * dist

        def probs(qm, km):
            s = jnp.einsum('bhqd,bhkd->bhqk', qm, km).astype(jnp.float32) * scale + bias
            return jax.nn.softmax(jnp.where(allowed, s, NEG_INF), axis=-1)

        a = probs(qblk[..., 0, :], k1) - lam * probs(qblk[..., 1, :], k2)
        return jnp.einsum('bhqk,bhkd->bhqd', a.astype(v.dtype), v)

    o = lax.map(block, (jnp.arange(nb), qb))
    return o.transpose(1, 2, 0, 3, 4).reshape(Bn, H, S, v.shape[-1])


def gated_delta_rule(q, k, v, g, beta):
    Bn, H, S, dk = q.shape
    dv = v.shape[-1]
    nc = S // CHUNK
    q = (q * dk ** -0.5).reshape(Bn, H, nc, CHUNK, dk)
    k = k.reshape(Bn, H, nc, CHUNK, dk)
    v = v.reshape(Bn, H, nc, CHUNK, dv)
    g = g.reshape(Bn, H, nc, CHUNK)
    beta = beta.reshape(Bn, H, nc, CHUNK)
    gcum = jnp.cumsum(g, axis=-1)
    tri = jnp.tril(jnp.ones((CHUNK, CHUNK), dtype=bool))
    strict = jnp.tril(jnp.ones((CHUNK, CHUNK), dtype=bool), -1)
    diff = gcum[..., :, None] - gcum[..., None, :]
    decay = jnp.where(tri, jnp.exp(jnp.where(tri, diff, 0.0)), 0.0)
    kk = jnp.einsum('bhnid,bhnjd->bhnij', k, k)
    lower = jnp.where(strict, beta[..., :, None] * kk * decay, 0.0)
    a_mat = jnp.eye(CHUNK, dtype=q.dtype) + lower
    rhs = jnp.concatenate([v * beta[..., None], k * (beta * jnp.exp(gcum))[..., None]], axis=-1)
    sol = lax.linalg.triangular_solve(a_mat, rhs, left_side=True, lower=True, unit_diagonal=True)
    u, w = sol[..., :dv], sol[..., dv:]
    qk = jnp.einsum('bhnid,bhnjd->bhnij', q, k) * decay
    q_dec = q * jnp.exp(gcum)[..., None]
    k_dec = k * jnp.exp(gcum[..., -1:] - gcum)[..., None]
    g_last = jnp.exp(gcum[..., -1])
    xs = tuple(jnp.moveaxis(t, 2, 0) for t in (u, w, qk, q_dec, k_dec, g_last))

    def step(state, inp):
        u_c, w_c, qk_c, qd_c, kd_c, gl_c = inp
        v_new = u_c - jnp.einsum('bhck,bhkv->bhcv', w_c, state)
        o = jnp.einsum('bhck,bhkv->bhcv', qd_c, state) + jnp.einsum('bhcs,bhsv->bhcv', qk_c, v_new)
        state = state * gl_c[..., None, None] + jnp.einsum('bhck,bhcv->bhkv', kd_c, v_new)
        return state, o

    s0 = jnp.zeros((Bn, H, dk, dv), q.dtype)
    _, o = lax.scan(step, s0, xs)
    return jnp.moveaxis(o, 0, 2).reshape(Bn, H, S, dv)


def hybrid_mixer(h, w_in, q_norm, k_norm, lam, lambda_init, out_norm_a, conv_w, a_log, dt_bias,
                 out_norm_b, w_branch_a, w_branch_b, w_out):
    Bn, S, _ = h.shape
    proj = h @ w_in
    da_q, da_k, da_v, g_q, g_k, g_v, g_z, g_b, g_a, gates = jnp.split(proj, split_points(IN_SPLITS), axis=-1)

    q = rms_norm(da_q.reshape(Bn, S, DA_HEADS, 2, DA_QK_DIM), q_norm).transpose(0, 2, 1, 3, 4)
    k = rms_norm(da_k.reshape(Bn, S, DA_HEADS, 2, DA_QK_DIM), k_norm).transpose(0, 2, 1, 3, 4)
    v = da_v.reshape(Bn, S, DA_HEADS, DA_V_DIM).transpose(0, 2, 1, 3)
    o_a = diff_attention(q, k, v, lam)
    o_a = rms_norm(o_a.transpose(0, 2, 1, 3), out_norm_a) * (1.0 - lambda_init)
    y_a = o_a.reshape(Bn, S, DA_WIDTH) @ w_branch_a

    qkv = jax.nn.silu(causal_conv(jnp.concatenate([g_q, g_k, g_v], axis=-1), conv_w))
    cq, ck, cv = jnp.split(qkv, [GDN_HEADS * GDN_K_DIM, 2 * GDN_HEADS * GDN_K_DIM], axis=-1)
    cq = l2_norm(cq.reshape(Bn, S, GDN_HEADS, GDN_K_DIM)).transpose(0, 2, 1, 3)
    ck = l2_norm(ck.reshape(Bn, S, GDN_HEADS, GDN_K_DIM)).transpose(0, 2, 1, 3)
    cv = cv.reshape(Bn, S, GDN_HEADS, GDN_V_DIM).astype(jnp.float32).transpose(0, 2, 1, 3)
    beta = jax.nn.sigmoid(g_b.astype(jnp.float32)).transpose(0, 2, 1)
    g = (-jnp.exp(a_log.astype(jnp.float32))
         * jax.nn.softplus(g_a.astype(jnp.float32) + dt_bias.astype(jnp.float32))).transpose(0, 2, 1)
    o_b = gated_delta_rule(cq, ck, cv, g, beta).astype(h.dtype)
    o_b = rms_norm(o_b.transpose(0, 2, 1, 3), out_norm_b) * jax.nn.silu(g_z.reshape(Bn, S, GDN_HEADS, GDN_V_DIM))
    y_b = o_b.reshape(Bn, S, GDN_WIDTH) @ w_branch_b

    gate = jax.nn.sigmoid(gates).reshape(Bn, S, N_BRANCH, D_MODEL)
    merged = gate[:, :, 0] * y_a + gate[:, :, 1] * y_b
    return merged @ w_out


def hier_moe(h, w_group, b_group, w_router, b_router, w1, w3, w2):
    Bn, S, D = h.shape
    N = Bn * S
    t = h.reshape(N, D)
    g_prob = jax.nn.softmax((t @ w_group).astype(jnp.float32) + b_group.astype(jnp.float32), axis=-1)
    g_top_p, g_top = lax.top_k(g_prob, 1)
    e_logits = ((t @ w_router).astype(jnp.float32) + b_router.astype(jnp.float32)).reshape(N, N_GROUPS, EXPERTS_PER_GROUP)
    e_in = jnp.take_along_axis(e_logits, g_top[:, :, None], axis=1)[:, 0]
    e_top_v, e_top_i = lax.top_k(e_in, TOP_K)
    gate = g_top_p * jax.nn.softmax(e_top_v, axis=-1)
    expert = g_top * EXPERTS_PER_GROUP + e_top_i

    A = N * TOP_K
    e_flat = expert.reshape(A)
    tok_flat = jnp.repeat(jnp.arange(N, dtype=jnp.int32), TOP_K)
    w_flat = gate.reshape(A)
    order = jnp.argsort(e_flat)
    e_sorted = e_flat[order]
    counts = jnp.bincount(e_flat, length=N_EXPERTS)
    padded = (counts + MOE_BLOCK - 1) // MOE_BLOCK * MOE_BLOCK
    start = jnp.cumsum(counts) - counts
    pstart = jnp.cumsum(padded) - padded
    pend = pstart + padded
    dest = pstart[e_sorted] + (jnp.arange(A) - start[e_sorted])
    cap = A + N_EXPERTS * MOE_BLOCK
    n_blocks = cap // MOE_BLOCK
    buf_tok = jnp.zeros((cap,), jnp.int32).at[dest].set(tok_flat[order])
    buf_w = jnp.zeros((cap,), jnp.float32).at[dest].set(w_flat[order])
    blk_start = jnp.arange(n_blocks) * MOE_BLOCK
    blk_expert = jnp.minimum(jnp.sum(pend[None, :] <= blk_start[:, None], axis=1), N_EXPERTS - 1)
    xb = t[buf_tok].reshape(n_blocks, MOE_BLOCK, D)

    def run(args):
        xblk, e = args
        hid = jax.nn.silu(xblk @ w1[e]) * (xblk @ w3[e])
        return hid @ w2[e]

    yb = lax.map(run, (xb, blk_expert)).reshape(cap, D) * buf_w[:, None].astype(t.dtype)
    y = jnp.zeros((N, D), t.dtype).at[buf_tok].add(yb)
    return y.reshape(Bn, S, D)


def setup_inputs(seed: int = 0) -> dict:
    key = jax.random.key(seed)
    ks = jax.random.split(key, 28)
    f32 = jnp.float32
    L, D = DEPTH, D_MODEL

    def nrm(k, shape, s):
        return jax.random.normal(k, shape, f32) * s

    dt = jnp.exp(jax.random.uniform(ks[15], (L, GDN_HEADS), f32, math.log(1e-3), math.log(1e-1)))
    return {
        'x': nrm(ks[0], (BATCH, SEQ, D), 1.0),
        'c': nrm(ks[1], (BATCH, D), 1.0),
        'w_ada': nrm(ks[2], (L, D, 6 * D), 0.5 * D ** -0.5),
        'b_ada': nrm(ks[3], (L, 6 * D), 0.02),
        'norm1_gain': 1.0 + nrm(ks[4], (L, D), 0.02),
        'w_in': nrm(ks[5], (L, D, D_IN), D ** -0.5),
        'da_q_norm': 1.0 + nrm(ks[6], (L, DA_QK_DIM), 0.02),
        'da_k_norm': 1.0 + nrm(ks[7], (L, DA_QK_DIM), 0.02),
        'da_lambda_q1': nrm(ks[8], (L, DA_QK_DIM), 0.1),
        'da_lambda_k1': nrm(ks[9], (L, DA_QK_DIM), 0.1),
        'da_lambda_q2': nrm(ks[10], (L, DA_QK_DIM), 0.1),
        'da_lambda_k2': nrm(ks[11], (L, DA_QK_DIM), 0.1),
        'da_out_norm': 1.0 + nrm(ks[12], (L, DA_V_DIM), 0.02),
        'gdn_conv': nrm(ks[13], (L, CONV_K, GDN_CONV_DIM), CONV_K ** -0.5),
        'gdn_a_log': jnp.log(jax.random.uniform(ks[14], (L, GDN_HEADS), f32, 1.0, 16.0)),
        'gdn_dt_bias': jnp.log(jnp.expm1(dt)),
        'gdn_out_norm': 1.0 + nrm(ks[16], (L, GDN_V_DIM), 0.02),
        'w_branch_a': nrm(ks[17], (L, DA_WIDTH, D), DA_WIDTH ** -0.5),
        'w_branch_b': nrm(ks[18], (L, GDN_WIDTH, D), GDN_WIDTH ** -0.5),
        'w_out': nrm(ks[19], (L, D, D), D ** -0.5),
        'norm2_gain': 1.0 + nrm(ks[20], (L, D), 0.02),
        'w_group': nrm(ks[21], (L, D, N_GROUPS), D ** -0.5),
        'b_group': nrm(ks[22], (L, N_GROUPS), 0.01),
        'w_router': nrm(ks[23], (L, D, N_EXPERTS), D ** -0.5),
        'b_router': nrm(ks[24], (L, N_EXPERTS), 0.01),
        'w1': nrm(ks[25], (L, N_EXPERTS, D, D_EXPERT), D ** -0.5),
        'w3': nrm(ks[26], (L, N_EXPERTS, D, D_EXPERT), D ** -0.5),
        'w2': nrm(ks[27], (L, N_EXPERTS, D_EXPERT, D), D_EXPERT ** -0.5),
    }


def reference(x, c, w_ada, b_ada, norm1_gain, w_in, da_q_norm, da_k_norm, da_lambda_q1, da_lambda_k1,
              da_lambda_q2, da_lambda_k2, da_out_norm, gdn_conv, gdn_a_log, gdn_dt_bias, gdn_out_norm,
              w_branch_a, w_branch_b, w_out, norm2_gain, w_group, b_group, w_router, b_router, w1, w3, w2):
    for layer in range(DEPTH):
        mod = (jax.nn.silu(c) @ w_ada[layer] + b_ada[layer])[:, None, :]
        shift1, scale1, gate1, shift2, scale2, gate2 = jnp.split(mod, 6, axis=-1)
        lambda_init = 0.8 - 0.6 * math.exp(-0.3 * layer)
        lam = (jnp.exp(jnp.sum(da_lambda_q1[layer].astype(jnp.float32) * da_lambda_k1[layer].astype(jnp.float32)))
               - jnp.exp(jnp.sum(da_lambda_q2[layer].astype(jnp.float32) * da_lambda_k2[layer].astype(jnp.float32)))
               + lambda_init)
        h = rms_norm(x, norm1_gain[layer]) * (1.0 + scale1) + shift1
        x = x + gate1 * hybrid_mixer(h, w_in[layer], da_q_norm[layer], da_k_norm[layer], lam, lambda_init,
                                     da_out_norm[layer], gdn_conv[layer], gdn_a_log[layer], gdn_dt_bias[layer],
                                     gdn_out_norm[layer], w_branch_a[layer], w_branch_b[layer], w_out[layer])
        h = rms_norm(x, norm2_gain[layer]) * (1.0 + scale2) + shift2
        x = x + gate2 * hier_moe(h, w_group[layer], b_group[layer], w_router[layer], b_router[layer],
                                 w1[layer], w3[layer], w2[layer])
    return x
```

```python
import math
from contextlib import ExitStack

import numpy as np
import ml_dtypes

import concourse.bass as bass
import concourse.mybir as mybir
from concourse.bass_utils import run_bass_kernel_spmd

F32 = mybir.dt.float32
BF16 = mybir.dt.bfloat16
I32 = mybir.dt.int32
AF = mybir.ActivationFunctionType
ALU = mybir.AluOpType
AX = mybir.AxisListType

D = 1024
SEQ = 8192
NTOK_C = 2048
EPS = 1e-6
NEG = -30000.0
LAMBDA_INIT = 0.8 - 0.6 * math.exp(-0.3 * 0)
NEXP = 32
DEXP = 512

ENGS = ("pe", "act", "dve", "pool", "sp")


class Buf:
    __slots__ = ("name", "last_w", "readers", "excl")

    def __init__(self, name, excl=False):
        self.name = name
        self.last_w = None
        self.readers = []
        self.excl = excl


class Op:
    __slots__ = ("eng", "fn", "deps", "is_dma", "is_cc", "dsem", "needs_inc", "val", "flushed")

    def __init__(self, eng, fn, is_dma=False, dsem=None, is_cc=False):
        self.eng = eng
        self.fn = fn
        self.deps = []
        self.is_dma = is_dma
        self.is_cc = is_cc
        self.dsem = dsem
        self.needs_inc = is_dma
        self.val = None
        self.flushed = False


class Sched:
    def __init__(self, nc):
        self.nc = nc
        self.ops = {e: [] for e in ENGS}
        self.dsem_count = {}
        self.last_on_eng = {e: None for e in ENGS}
        self.last_dma = {}
        self.pending_barrier = {e: [] for e in ENGS}
        self.nops = 0

    def _add_deps(self, op, reads, writes):
        deps = []
        xr = [b for b in reads if b.excl]
        if xr:
            reads = [b for b in reads if not b.excl]
            writes = list(writes) + [b for b in xr if b not in writes]
        for b in reads:
            if b.last_w is not None:
                deps.append(b.last_w)
        for b in writes:
            if b.last_w is not None:
                deps.append(b.last_w)
            deps.extend(b.readers)
        deps.extend(self.pending_barrier[op.eng])
        self.pending_barrier[op.eng] = []
        seen = set()
        for d in deps:
            if d is op or id(d) in seen:
                continue
            seen.add(id(d))
            if (not d.is_dma) and (not op.is_dma) and d.eng == "pe" and op.eng == "pe":
                continue
            if d.flushed and d.val is None:
                continue
            op.deps.append(d)
            d.needs_inc = True
        for b in reads:
            b.readers.append(op)
        for b in writes:
            b.last_w = op
            b.readers = []

    def op(self, eng, fn, reads=(), writes=()):
        o = Op(eng, fn)
        self._add_deps(o, reads, writes)
        self.ops[eng].append(o)
        self.last_on_eng[eng] = o
        self.nops += 1
        return o

    def dma(self, queue, pairs, reads=(), writes=(), dsem=None):
        if dsem is None:
            dsem = writes[0].name if writes else reads[0].name
        o = Op(queue, pairs, is_dma=True, dsem=dsem)
        self._add_deps(o, reads, writes)
        self.dsem_count[dsem] = self.dsem_count.get(dsem, 0) + 16 * len(pairs)
        o.val = self.dsem_count[dsem]
        self.ops[queue].append(o)
        self.last_dma[dsem] = o
        self.nops += 1
        return o

    def cc(self, fn, reads=(), writes=(), dsem="cc"):
        o = Op("pool", fn, is_dma=True, dsem=dsem, is_cc=True)
        self._add_deps(o, reads, writes)
        assert dsem not in self.dsem_count
        self.dsem_count[dsem] = 1
        o.val = 1
        self.ops["pool"].append(o)
        self.last_dma[dsem] = o
        return o

    def barrier(self):
        targets = [o for o in self.last_on_eng.values() if o is not None and not o.is_dma]
        targets += list(self.last_dma.values())
        for e in ENGS:
            self.pending_barrier[e] = list(targets)

    def flush(self, stack, final=False, final_waits_engine="sp"):
        nc = self.nc
        self.barrier()
        fin = self.pending_barrier[final_waits_engine] if final else []
        for e in ENGS:
            for d in self.pending_barrier[e]:
                d.needs_inc = True
        if not hasattr(self, "esem"):
            self.esem = {e: stack.enter_context(nc.semaphore("s_" + e)) for e in ENGS}
            self.dsem = {}
            self.ecount = {e: 0 for e in ENGS}
            self.waited = {e: {} for e in ENGS}
        for e in ENGS:
            c = self.ecount[e]
            for o in self.ops[e]:
                if not o.is_dma and o.needs_inc:
                    c += 1
                    o.val = c
            self.ecount[e] = c
        for k in self.dsem_count:
            if k not in self.dsem:
                self.dsem[k] = stack.enter_context(nc.semaphore("d_%d" % len(self.dsem)))
        esem, dsem = self.esem, self.dsem

        def sem_of(d):
            return (dsem[d.dsem], d.val) if d.is_dma else (esem[d.eng], d.val)

        ops = self.ops
        self.ops = {e: [] for e in ENGS}
        with nc.Block() as block:

            def run(engname, eng):
                waited = self.waited[engname]
                for o in ops[engname]:
                    for d in o.deps:
                        s, v = sem_of(d)
                        key = id(s)
                        if waited.get(key, 0) >= v:
                            continue
                        waited[key] = v
                        eng.wait_ge(s, v)
                    if o.is_cc:
                        o.fn(eng).then_inc(dsem[o.dsem])
                    elif o.is_dma:
                        for (out_ap, in_ap) in o.fn:
                            if callable(out_ap):
                                out_ap = out_ap(eng)
                            if callable(in_ap):
                                in_ap = in_ap(eng)
                            eng.dma_start(out=out_ap, in_=in_ap).then_inc(dsem[o.dsem], 16)
                    else:
                        inst = o.fn(eng)
                        if o.needs_inc:
                            inst.then_inc(esem[engname], 1)
                    o.fn = None
                    o.flushed = True
                if final and engname == final_waits_engine:
                    for d in fin:
                        s, v = sem_of(d)
                        if waited.get(id(s), 0) >= v:
                            continue
                        waited[id(s)] = v
                        eng.wait_ge(s, v)

            @block.sync
            def _(eng):
                run("sp", eng)

            @block.tensor
            def _(eng):
                run("pe", eng)

            @block.scalar
            def _(eng):
                run("act", eng)

            @block.vector
            def _(eng):
                run("dve", eng)

            @block.gpsimd
            def _(eng):
                run("pool", eng)


class K:
    def __init__(self, nc, S):
        self.nc = nc
        self.S = S

    def mm(self, out, lhsT, rhs, start, stop, reads, writes, sgc=False):
        if sgc:
            self.S.op("pe", lambda e: e.matmul(out, lhsT=lhsT, rhs=rhs, start=start, stop=stop, skip_group_check=True),
                      reads, writes)
        else:
            self.S.op("pe", lambda e: e.matmul(out, lhsT=lhsT, rhs=rhs, start=start, stop=stop), reads, writes)

    def tr(self, out, in_, ident, reads, writes):
        self.S.op("pe", lambda e: e.transpose(out, in_, ident), reads, writes)

    def act(self, out, in_, func, reads, writes, scale=None, bias=None, accum_out=None, eng="act"):
        kw = {}
        if scale is not None:
            kw["scale"] = scale
        if bias is not None:
            kw["bias"] = bias
        if accum_out is not None:
            kw["accum_out"] = accum_out
        self.S.op(eng, lambda e: e.activation(out=out, in_=in_, func=func, **kw), reads, writes)

    def tt(self, out, in0, in1, op, reads, writes, eng="dve"):
        self.S.op(eng, lambda e: e.tensor_tensor(out=out, in0=in0, in1=in1, op=op), reads, writes)

    def ts(self, out, in0, s1, op0, reads, writes, s2=None, op1=None, eng="dve", accum_out=None):
        kw = {}
        if op1 is not None:
            kw["op1"] = op1
        if accum_out is not None:
            kw["accum_out"] = accum_out
        self.S.op(eng, lambda e: e.tensor_scalar(out=out, in0=in0, scalar1=s1, scalar2=s2, op0=op0, **kw), reads, writes)

    def stt(self, out, in0, scalar, in1, op0, op1, reads, writes):
        self.S.op("dve", lambda e: e.scalar_tensor_tensor(out=out, in0=in0, scalar=scalar, in1=in1, op0=op0, op1=op1),
                  reads, writes)

    def copy(self, out, in_, reads, writes, eng="dve"):
        if eng == "act":
            self.S.op("act", lambda e: e.copy(out=out, in_=in_), reads, writes)
        else:
            self.S.op(eng, lambda e: e.tensor_copy(out=out, in_=in_), reads, writes)

    def recip(self, out, in_, reads, writes):
        self.S.op("dve", lambda e: e.reciprocal(out=out, in_=in_), reads, writes)

    def memset(self, ap, val, writes, eng="pool"):
        self.S.op(eng, lambda e: e.memset(ap, val), (), writes)

    def dma(self, q, out, in_, reads, writes, dsem=None):
        self.S.dma(q, [(out, in_)], reads, writes, dsem)


def build_program(dbg=None):
    dbg = dbg or {}
    nc = bass.Bass("TRN2", target_bir_lowering=False)
    S = Sched(nc)
    k = K(nc, S)

    def din(name, shape, dt=F32):
        return nc.dram_tensor(name, list(shape), dt, kind="ExternalInput").ap()

    xb = din("xb", [SEQ, D])
    xs = din("xs", [NTOK_C, D])
    cT = din("cT", [128, 8])
    w_ada = din("w_ada", [D, 6 * D])
    b_ada = din("b_ada", [1, 6 * D])
    gain1 = din("gain1", [1, D])
    gain2 = din("gain2", [1, D])
    wA = din("wA", [D, 898])
    wG = din("wG", [D, 2048])
    qkg = din("qkg", [64, 2])
    lamv = din("lamv", [64, 4])
    onAc = din("onAc", [128, 1])
    onB = din("onB", [1, 128])
    convw = din("convw", [128, 12])
    adt = din("adt", [1, 2])
    w_ba = din("w_ba", [512, D])
    w_bb = din("w_bb", [512, D])
    w_out = din("w_out", [D, D])
    w_rt = din("w_rt", [D, 36])
    b_rt = din("b_rt", [1, 36])
    w1 = din("w1", [NEXP, D, DEXP])
    w3 = din("w3", [NEXP, D, DEXP])
    w2 = din("w2", [NEXP, DEXP, D])
    c_ident = din("c_ident", [128, 128])
    c_aug_q = din("c_aug_q", [3, SEQ])
    c_aug_k = din("c_aug_k", [3, SEQ])
    c_bias = din("c_bias", [128, 64])
    c_dmask = din("c_dmask", [128, 4 * 512])
    c_gmask = din("c_gmask", [128, 7 * 128])
    out = nc.dram_tensor("out", [NTOK_C, D], F32, kind="ExternalOutput").ap()

    exA_in = nc.dram_tensor("exA_in", [4, 128, NTOK_C], BF16)
    exB_in = nc.dram_tensor("exB_in", [4, 128, NTOK_C], BF16)
    if dbg.get("ex_out_input"):
        exA_out = nc.dram_tensor("exA_out", [4, 512, NTOK_C], BF16, kind="ExternalInput")
        exB_out = nc.dram_tensor("exB_out", [4, 512, NTOK_C], BF16, kind="ExternalInput")
    else:
        exA_out = nc.dram_tensor("exA_out", [4, 512, NTOK_C], BF16)
        exB_out = nc.dram_tensor("exB_out", [4, 512, NTOK_C], BF16)
    x1d = nc.dram_tensor("x1d", [NTOK_C, D], F32).ap()
    gdn_d = nc.dram_tensor("gdn_d", [3, 128, SEQ], BF16).ap()
    z_d = nc.dram_tensor("z_d", [SEQ, 128], F32).ap()
    dbg_outs = {}

    def dbg_out(name, shape, dt=F32):
        dbg_outs[name] = nc.dram_tensor(name, list(shape), dt, kind="ExternalOutput").ap()
        return dbg_outs[name]

    B_exA_in = [Buf("exA_in%d" % i) for i in range(4)]
    B_exB_in = [Buf("exB_in%d" % i) for i in range(4)]
    B_exA_out = Buf("exA_out")
    B_exB_out = Buf("exB_out")
    B_x1d = Buf("x1d")
    B_out = Buf("outd")

    with ExitStack() as st:
        def sb(stack, name, shape, dt=F32):
            return stack.enter_context(nc.sbuf_tensor(name, list(shape), dt))

        MOD = sb(st, "MOD", [128, 6 * D])
        B_MOD = Buf("MOD")
        identf = sb(st, "identf", [128, 128])
        identb = sb(st, "identb", [128, 128], BF16)
        onesf = sb(st, "onesf", [128, 128])
        epsc = sb(st, "epsc", [128, 1])
        neglam = sb(st, "neglam", [128, 1])
        B_const = Buf("const")
        B_neglam = Buf("neglam")
        BA = sb(st, "BA", [128, 64, 2])
        B_BA = Buf("BA")
        psb = [st.enter_context(nc.psum_tensor("ps%d" % i, [128, 512], F32)) for i in range(8)]
        B_ps = [Buf("ps%d" % i, excl=True) for i in range(8)]

        SH1 = MOD[:, 0:1024]
        G1 = MOD[:, 1024:2048]
        GATE1 = MOD[:, 2048:3072]
        SH2 = MOD[:, 3072:4096]
        G2 = MOD[:, 4096:5120]
        GATE2 = MOD[:, 5120:6144]

        with ExitStack() as s0:
            k.dma("sp", identf[:], c_ident, (), [B_const], dsem="c0")
            k.memset(onesf[:], 1.0, [B_const])
            k.memset(epsc[:], EPS, [B_const])
            k.copy(identb[:], identf[:], [B_const], [B_const], eng="dve")
            cv = sb(s0, "cv", [128, 8])
            sc = sb(s0, "sc", [128, 8])
            scb = sb(s0, "scb", [128, 8, 128])
            wst = [sb(s0, "wst%d" % i, [128, 8, 512]) for i in range(4)]
            g1t = sb(s0, "g1t", [128, D])
            g2t = sb(s0, "g2t", [128, D])
            lv = sb(s0, "lv", [64, 4])
            lp = sb(s0, "lp", [64, 2])
            le = sb(s0, "le", [128, 2])
            B_cv, B_sc, B_scb, B_g1t, B_g2t, B_lv, B_lp, B_le = [Buf(n) for n in
                                                              "cv sc scb g1t g2t lv lp le".split()]
            B_wst = [Buf("wst%d" % i) for i in range(4)]
            k.dma("sp", cv[:], cT, (), [B_cv])
            k.dma("sp", MOD[:], b_ada.partition_broadcast(128), (), [B_MOD])
            k.dma("sp", g1t[:], gain1.partition_broadcast(128), (), [B_g1t])
            k.dma("sp", g2t[:], gain2.partition_broadcast(128), (), [B_g2t])
            k.dma("sp", lv[:], lamv, (), [B_lv])
            k.act(sc[:], cv[:], AF.Silu, [B_cv], [B_sc])
            for kc in range(8):
                k.ts(scb[:, kc, :], onesf[:], sc[:, kc:kc + 1], ALU.mult, [B_sc, B_const], [B_scb])
            w_ada_v = w_ada.rearrange("(kc p) n -> p kc n", p=128)
            def ada_load(nb):
                k.dma("sp" if nb % 2 == 0 else "act", wst[nb % 4][:], w_ada_v[:, :, nb * 512:(nb + 1) * 512], (), [B_wst[nb % 4]])
            for nb in range(3):
                ada_load(nb)
            for nb in range(12):
                wb = nb % 4
                if nb + 3 < 12:
                    ada_load(nb + 3)
                pb = nb % 2
                for kc in range(8):
                    k.mm(psb[pb][:, :], scb[:, kc, :], wst[wb][:, kc, :], kc == 0, kc == 7,
                         [B_scb, B_wst[wb]], [B_ps[pb]])
                k.tt(MOD[:, nb * 512:(nb + 1) * 512], psb[pb][:, :], MOD[:, nb * 512:(nb + 1) * 512], ALU.add,
                     [B_ps[pb], B_MOD], [B_MOD])
            k.stt(G1, G1, 1.0, g1t[:], ALU.add, ALU.mult, [B_MOD, B_g1t], [B_MOD])
            k.stt(G2, G2, 1.0, g2t[:], ALU.add, ALU.mult, [B_MOD, B_g2t], [B_MOD])
            k.tt(lp[:, 0:1], lv[:, 0:1], lv[:, 1:2], ALU.mult, [B_lv], [B_lp])
            k.tt(lp[:, 1:2], lv[:, 2:3], lv[:, 3:4], ALU.mult, [B_lv], [B_lp])
            k.mm(psb[2][:, 0:2], onesf[0:64, :], lp[:, :], True, True, [B_lp, B_const], [B_ps[2]])
            k.act(le[:], psb[2][:, 0:2], AF.Exp, [B_ps[2]], [B_le])
            k.tt(neglam[:], le[:, 1:2], le[:, 0:1], ALU.subtract, [B_le], [B_neglam])
            k.ts(neglam[:], neglam[:], -LAMBDA_INIT, ALU.add, [B_neglam], [B_neglam])
            if dbg.get("out_mod"):
                o = dbg_out("d_mod", [128, 6 * D])
                k.dma("sp", o, MOD[:], [B_MOD], [Buf("d_mod")])
                o2 = dbg_out("d_lam", [128, 1])
                k.dma("sp", o2, neglam[:], [B_neglam], [Buf("d_lam")])
            S.flush(st)

        ctx = dict(nc=nc, S=S, k=k, st=st, sb=sb, MOD=MOD, B_MOD=B_MOD, identf=identf, identb=identb, onesf=onesf,
                   epsc=epsc, neglam=neglam, B_const=B_const, B_neglam=B_neglam, psb=psb, B_ps=B_ps,
                   BA=BA, B_BA=B_BA, B_gdn=Buf("gdn_d"), B_zd=Buf("z_d"),
                   SH1=SH1, G1=G1, GATE1=GATE1, SH2=SH2, G2=G2, GATE2=GATE2, dbg=dbg, dbg_out=dbg_out)
        io = dict(xb=xb, xs=xs, wA=wA, wG=wG, qkg=qkg, onAc=onAc, onB=onB, convw=convw, adt=adt, w_ba=w_ba, w_bb=w_bb,
                  w_out=w_out, w_rt=w_rt, b_rt=b_rt, w1=w1, w3=w3, w2=w2, c_aug_q=c_aug_q, c_aug_k=c_aug_k,
                  c_bias=c_bias, c_dmask=c_dmask, c_gmask=c_gmask, out=out, exA_in=exA_in, exB_in=exB_in, exA_out=exA_out,
                  exB_out=exB_out, x1d=x1d, gdn_d=gdn_d, z_d=z_d, B_exA_in=B_exA_in, B_exB_in=B_exB_in,
                  B_exA_out=B_exA_out, B_exB_out=B_exB_out, B_x1d=B_x1d, B_out=B_out)

        def gather1(ein, eout, B_in, B_o, tag, sl):
            def ccfn(e):
                return e.collective_compute("AllGather", ALU.bypass, replica_groups=[[0, 1, 2, 3], [4, 5, 6, 7]],
                                            ins=[ein.ap()[sl].opt()], outs=[eout.ap()[sl].opt()])
            S.cc(ccfn, [B_in[sl]], [B_o], dsem="cc_%s%d" % (tag, sl))

        if not dbg.get("ex_out_input"):
            ctx["gatherA"] = lambda sl: gather1(exA_in, exA_out, B_exA_in, B_exA_out, "a", sl)
            ctx["gatherB"] = lambda sl: gather1(exB_in, exB_out, B_exB_in, B_exB_out, "b", sl)
        if not dbg.get("skip_p1"):
            phase1(ctx, io)
        if not dbg.get("skip_p2"):
            phase2(ctx, io)
        if dbg.get("out_gdn"):
            o = dbg_out("d_gdn", [3, 128, SEQ], BF16)
            k.dma("sp", o, gdn_d, [ctx["B_gdn"]], [Buf("d_gdn")])
            o = dbg_out("d_z", [SEQ, 128])
            k.dma("sp", o, z_d, [ctx["B_zd"]], [Buf("d_z")])
            o = dbg_out("d_ba", [128, 128])
            k.dma("sp", o, BA[:].rearrange("p a b -> p (a b)"), [B_BA], [Buf("d_ba")])
        if dbg.get("out_exin"):
            o = dbg_out("d_exin", [2, 4, 128, NTOK_C], BF16)
            k.dma("sp", o[0], exA_in.ap(), B_exA_in, [Buf("d_exinA")])
            k.dma("sp", o[1], exB_in.ap(), B_exB_in, [Buf("d_exinB")])
        if not dbg.get("skip_p3"):
            phase3(ctx, io)
        S.flush(st, final=True)
    return nc, dbg_outs


def norm_mod(ctx, xt, B_xt, G, SH, junk, B_junk, ssq, B_ssq, tmp, B_tmp, out_ap, B_out, out_eng="dve"):
    k = ctx["k"]
    k.act(junk, xt, AF.Square, [B_xt], [B_junk, B_ssq], accum_out=ssq[:, 0:1])
    k.act(ssq[:, 1:2], ssq[:, 0:1], AF.Ln, [B_ssq], [B_ssq], scale=1.0 / D, bias=ctx["epsc"][:, 0:1])
    k.act(ssq[:, 2:3], ssq[:, 1:2], AF.Exp, [B_ssq], [B_ssq], scale=-0.5)
    k.stt(tmp, xt, ssq[:, 2:3], G, ALU.mult, ALU.mult, [B_xt, B_ssq, ctx["B_MOD"]], [B_tmp])
    k.tt(out_ap, tmp, SH, ALU.add, [B_tmp, ctx["B_MOD"]], [B_out], eng=out_eng)


def transpose_block_bf(ctx, hb, B_hb, dst, B_dst, pbank, evac_eng):
    k = ctx["k"]
    ps = ctx["psb"][pbank]
    B_p = ctx["B_ps"][pbank]
    pv = ps[:, :].bitcast(BF16)
    for kc in range(8):
        k.tr(pv[:, kc * 128:(kc + 1) * 128], hb[:, kc * 128:(kc + 1) * 128], ctx["identb"][:], [B_hb, ctx["B_const"]], [B_p])
    src = pv.rearrange("p (kc t) -> p kc t", kc=8)
    if evac_eng == "act":
        k.copy(dst, src, [B_p], [B_dst], eng="act")
    else:
        k.copy(dst, src, [B_p], [B_dst], eng=evac_eng)


def phase3(ctx, io):
    nc, S, k, st, sb = ctx["nc"], ctx["S"], ctx["k"], ctx["st"], ctx["sb"]
    psb, B_ps = ctx["psb"], ctx["B_ps"]
    dbg = ctx["dbg"]
    exA2 = io["exA_out"].ap().rearrange("s r n -> (s r) n")
    exB2 = io["exB_out"].ap().rearrange("s r n -> (s r) n")
    with ExitStack() as s3:
        H2T = sb(s3, "H2T", [128, 8, NTOK_C], BF16)
        B_H2T = [Buf("H2T%d" % i) for i in range(16)]
        GW = sb(s3, "GW", [128, 16, 32])
        B_GW = [Buf("GW%d" % i) for i in range(16)]
        with ExitStack() as s31:
            WGt = [sb(s31, "WGt%d" % i, [128, 8, 512], BF16) for i in range(2)]
            B_WGt = [Buf("WGt0"), Buf("WGt1")]
            WAt = sb(s31, "WAt", [128, 4, D], BF16)
            WBt = sb(s31, "WBt", [128, 4, D], BF16)
            WOt = sb(s31, "WOt", [128, 8, D], BF16)
            WRt = sb(s31, "WRt", [128, 8, 36])
            brt = sb(s31, "brt", [128, 36])
            B_W = Buf("p3w")
            XT = sb(s31, "XT", [128, 4, D])
            B_XT = [Buf("XT%d" % i) for i in range(4)]
            junk = sb(s31, "junk3", [128, D], BF16)
            B_junk = Buf("junk3")
            ssq = sb(s31, "ssq3", [128, 4])
            B_ssq = Buf("ssq3")
            hT = sb(s31, "hT3", [128, 8, 512], BF16)
            B_hT = Buf("hT3")
            GT = sb(s31, "GT", [128, 16, 512], BF16)
            B_GT = Buf("GT")
            OAB = sb(s31, "OAB", [128, 4, 2, 512], BF16)
            B_OAB = Buf("OAB")
            MT = sb(s31, "MT", [128, 8, 512], BF16)
            B_MT = Buf("MT")
            t1 = t2 = B_t1 = B_t2 = None
            x1t = [sb(s31, "x1t%d" % i, [128, D]) for i in range(2)]
            B_x1t = [Buf("x1t0"), Buf("x1t1")]
            B_rl = Buf("rl")
            B_rw = Buf("rw")
            def mk2(name, shape, dt=F32):
                return [sb(s31, "%s_%d" % (name, i), shape, dt) for i in range(2)], [Buf("%s_%d" % (name, i)) for i in range(2)]
            tmpf2, B_tmpf2 = mk2("tmpf2", [128, D])
            junk2, B_junk2 = [junk, junk], [B_junk, Buf("junk3b")]
            ssq2, B_ssq2 = mk2("ssq2", [128, 4])
            ssq4 = sb(s31, "ssq4_3", [128, 4, 4])
            B_ssq4 = [Buf("ssq4_3_%d" % i) for i in range(4)]
            h2f2, B_h2f2 = mk2("h2f2", [128, D])
            h2b2, B_h2b2 = mk2("h2b2", [128, D], BF16)
            h2T322, B_h2T322 = mk2("h2T322", [128, 8, 128])
            t1 = [tmpf2[1][:, 0:512], tmpf2[0][:, 0:512]]
            t2 = [tmpf2[1][:, 512:1024], tmpf2[0][:, 512:1024]]
            B_t1 = [B_tmpf2[1], B_tmpf2[0]]
            B_t2 = [B_tmpf2[1], B_tmpf2[0]]
            hb, B_hb = h2b2, B_h2b2
            tmpf, B_tmpf = tmpf2[0], B_tmpf2[0]
            rl2, B_rl2 = mk2("rl2", [128, 36])
            rw2, B_rw2 = mk2("rw2", [128, 160])

            k.dma("pool", WAt[:], io["w_ba"].rearrange("(h p) n -> p h n", p=128), (), [B_W], dsem="p3w_a")
            k.dma("pool", WBt[:], io["w_bb"].rearrange("(h p) n -> p h n", p=128), (), [B_W], dsem="p3w_b")
            k.dma("pool", WOt[:], io["w_out"].rearrange("(kc p) n -> p kc n", p=128), (), [B_W], dsem="p3w_o")
            k.dma("sp", WRt[:], io["w_rt"].rearrange("(kc p) n -> p kc n", p=128), (), [B_W], dsem="p3w_r")
            k.dma("sp", brt[:], io["b_rt"].partition_broadcast(128), (), [B_W], dsem="p3w_rb")
            wG_v = io["wG"].rearrange("(kc p) n -> p kc n", p=128)
            wgi = 0
            jcache = {}
            for t in range(4):
                for blk in range(4):
                    gb = 4 * t + blk
                    k.dma("sp", XT[:, blk, :], io["xs"][gb * 128:(gb + 1) * 128, :], (), [B_XT[blk]])
                for blk in range(4):
                    k.act(junk[:], XT[:, blk, :], AF.Square, [B_XT[blk]], [B_junk, B_ssq4[blk]], accum_out=ssq4[:, blk, 0:1])
                for blk in range(4):
                    k.act(ssq4[:, blk, 1:2], ssq4[:, blk, 0:1], AF.Ln, [B_ssq4[blk]], [B_ssq4[blk]], scale=1.0 / D, bias=ctx["epsc"][:, 0:1])
                for blk in range(4):
                    k.act(ssq4[:, blk, 2:3], ssq4[:, blk, 1:2], AF.Exp, [B_ssq4[blk]], [B_ssq4[blk]], scale=-0.5)
                for blk in range(4):
                    hbi = blk % 2
                    k.stt(tmpf[:], XT[:, blk, :], ssq4[:, blk, 2:3], ctx["G1"], ALU.mult, ALU.mult, [B_XT[blk], B_ssq4[blk], ctx["B_MOD"]], [B_tmpf])
                    k.tt(hb[hbi][:], tmpf[:], ctx["SH1"], ALU.add, [B_tmpf, ctx["B_MOD"]], [B_hb[hbi]])
                    transpose_block_bf(ctx, hb[hbi], B_hb[hbi], hT[:, :, blk * 128:(blk + 1) * 128], B_hT,
                                       pbank=blk % 2, evac_eng="act" if blk % 2 == 0 else "dve")
                for gq in range(4):
                    wb = wgi % 2
                    wgi += 1
                    k.dma("pool", WGt[wb][:], wG_v[:, :, gq * 512:(gq + 1) * 512], (), [B_WGt[wb]])
                    for gc in range(4):
                        pb = 2 + (gc % 2)
                        for kc in range(8):
                            k.mm(psb[pb][:, :], WGt[wb][:, kc, gc * 128:(gc + 1) * 128], hT[:, kc, :], kc == 0, kc == 7,
                                 [B_WGt[wb], B_hT], [B_ps[pb]])
                        k.act(GT[:, gq * 4 + gc, :], psb[pb][:, :], AF.Sigmoid, [B_ps[pb]], [B_GT])
                pairs = []
                for r_ in range(4):
                    for two, ex2 in enumerate((exA2, exB2)):
                        def src_fn(eng, t=t, r_=r_, ex2=ex2):
                            if "j" not in jcache:
                                jcache["j"] = eng.partition_id() % 4
                            return ex2[bass.ds(jcache["j"] * 512 + r_ * 128, 128), t * 512:(t + 1) * 512]
                        pairs.append((OAB[:, r_, two, :], src_fn))
                S.dma("pool", pairs, [io["B_exA_out"], io["B_exB_out"]], [B_OAB])
                for dc in range(8):
                    i2 = dc % 2
                    pa_, pb_ = (4, 5) if i2 == 0 else (6, 7)
                    for r in range(4):
                        k.mm(psb[pa_][:, :], WAt[:, r, dc * 128:(dc + 1) * 128], OAB[:, r, 0, :], r == 0, r == 3,
                             [B_W, B_OAB], [B_ps[pa_]])
                    for r in range(4):
                        k.mm(psb[pb_][:, :], WBt[:, r, dc * 128:(dc + 1) * 128], OAB[:, r, 1, :], r == 0, r == 3,
                             [B_W, B_OAB], [B_ps[pb_]])
                    k.tt(t1[i2], GT[:, dc, :], psb[pa_][:, :], ALU.mult, [B_GT, B_ps[pa_]], [B_t1[i2]])
                    k.tt(t2[i2], GT[:, 8 + dc, :], psb[pb_][:, :], ALU.mult, [B_GT, B_ps[pb_]], [B_t2[i2]])
                    k.tt(MT[:, dc, :], t1[i2], t2[i2], ALU.add, [B_t1[i2], B_t2[i2]], [B_MT])
                def blockchain(blk, sl):
                    gb = 4 * t + blk
                    pbo = 4 + 2 * sl
                    for nb in range(2):
                        pb = pbo + nb
                        for dc in range(8):
                            k.mm(psb[pb][:, :], MT[:, dc, blk * 128:(blk + 1) * 128], WOt[:, dc, nb * 512:(nb + 1) * 512],
                                 dc == 0, dc == 7, [B_MT, B_W], [B_ps[pb]])
                        yield
                        k.tt(tmpf2[sl][:, nb * 512:(nb + 1) * 512], psb[pb][:, :], ctx["GATE1"][:, nb * 512:(nb + 1) * 512], ALU.mult,
                             [B_ps[pb], ctx["B_MOD"]], [B_tmpf2[sl]])
                        yield
                    k.tt(x1t[sl][:], tmpf2[sl][:], XT[:, blk, :], ALU.add, [B_tmpf2[sl], B_XT[blk]], [B_x1t[sl]])
                    k.dma("sp", io["x1d"][gb * 128:(gb + 1) * 128, :], x1t[sl][:], [B_x1t[sl]], [io["B_x1d"]], dsem="x1d_w%d" % sl)
                    yield
                    k.act(junk2[sl][:], x1t[sl][:], AF.Square, [B_x1t[sl]], [B_junk2[sl], B_ssq2[sl]], accum_out=ssq2[sl][:, 0:1])
                    yield
                    k.act(ssq2[sl][:, 1:2], ssq2[sl][:, 0:1], AF.Ln, [B_ssq2[sl]], [B_ssq2[sl]], scale=1.0 / D, bias=ctx["epsc"][:, 0:1])
                    k.act(ssq2[sl][:, 2:3], ssq2[sl][:, 1:2], AF.Exp, [B_ssq2[sl]], [B_ssq2[sl]], scale=-0.5)
                    yield
                    k.stt(tmpf2[sl][:], x1t[sl][:], ssq2[sl][:, 2:3], ctx["G2"], ALU.mult, ALU.mult, [B_x1t[sl], B_ssq2[sl], ctx["B_MOD"]],
                          [B_tmpf2[sl]])
                    yield
                    k.tt(h2f2[sl][:], tmpf2[sl][:], ctx["SH2"], ALU.add, [B_tmpf2[sl], ctx["B_MOD"]], [B_h2f2[sl]])
                    yield
                    k.copy(h2b2[sl][:], h2f2[sl][:], [B_h2f2[sl]], [B_h2b2[sl]], eng="act")
                    yield
                    pv = psb[sl][:, :].bitcast(BF16)
                    for kc in range(8):
                        k.tr(pv[:, kc * 128:(kc + 1) * 128], h2b2[sl][:, kc * 128:(kc + 1) * 128], ctx["identb"][:],
                             [B_h2b2[sl], ctx["B_const"]], [B_ps[sl]])
                    yield
                    k.copy(H2T[:, :, gb * 128:(gb + 1) * 128], pv.rearrange("p (kc t) -> p kc t", kc=8), [B_ps[sl]], [B_H2T[gb]], eng="act")
                    yield
                    for half in range(2):
                        pb = 2 + sl
                        for q4 in range(4):
                            kc = half * 4 + q4
                            k.tr(psb[pb][:, q4 * 128:(q4 + 1) * 128], h2f2[sl][:, kc * 128:(kc + 1) * 128], ctx["identf"][:],
                                 [B_h2f2[sl], ctx["B_const"]], [B_ps[pb]])
                        yield
                        k.copy(h2T322[sl][:, half * 4:(half + 1) * 4, :], psb[pb][:, :].rearrange("p (a b) -> p a b", a=4),
                               [B_ps[pb]], [B_h2T322[sl]], eng="dve")
                        yield
                    for kc in range(8):
                        k.mm(psb[pbo][:, 0:36], h2T322[sl][:, kc, :], WRt[:, kc, :], kc == 0, kc == 7, [B_h2T322[sl], B_W], [B_ps[pbo]])
                    yield
                    k.tt(rl2[sl][:], psb[pbo][:, 0:36], brt[:], ALU.add, [B_ps[pbo], B_W], [B_rl2[sl]])
                    yield
                    yield from routing(ctx, rl2[sl], B_rl2[sl], rw2[sl], B_rw2[sl], GW[:, gb, :], B_GW[gb])

                for pair in range(2):
                    gens = [blockchain(2 * pair, 0), blockchain(2 * pair + 1, 1)]
                    alive = [True, True]
                    while any(alive):
                        for gi_ in range(2):
                            if alive[gi_]:
                                try:
                                    next(gens[gi_])
                                except StopIteration:
                                    alive[gi_] = False
            S.flush(st)
        if dbg.get("out_gw"):
            o = ctx["dbg_out"]("d_gw", [128, 16 * 32])
            k.dma("sp", o, GW[:].rearrange("p a b -> p (a b)"), B_GW, [Buf("d_gw")])
        with ExitStack() as s32:
            ACC = sb(s32, "ACC", [128, 16, D])
            B_ACC = [Buf("ACC%d" % i) for i in range(16)]
            EW1 = [sb(s32, "EW1_%d" % i, [128, 8, DEXP], BF16) for i in range(2)]
            EW3 = [sb(s32, "EW3_%d" % i, [128, 8, DEXP], BF16) for i in range(2)]
            EW2 = [sb(s32, "EW2_%d" % i, [128, 4, D], BF16) for i in range(2)]
            B_EW1 = [Buf("EW1_0"), Buf("EW1_1")]
            B_EW3 = [Buf("EW3_0"), Buf("EW3_1")]
            B_EW2 = [Buf("EW2_0"), Buf("EW2_1")]
            HID = [sb(s32, "HID%d" % i, [128, 4, 512], BF16) for i in range(2)]
            B_HID = [Buf("HID0"), Buf("HID1")]
            SIL = [sb(s32, "SIL%d" % i, [128, 512]) for i in range(2)]
            B_SIL = [Buf("SIL0"), Buf("SIL1")]
            xo = [sb(s32, "xo%d" % i, [128, D]) for i in range(2)]
            B_xo = [Buf("xo0"), Buf("xo1")]
            for e in range(NEXP):
                eb = e % 2
                k.dma("pool", EW1[eb][:], io["w1"][e].rearrange("(kc p) n -> p kc n", p=128), (), [B_EW1[eb]])
                k.dma("pool", EW3[eb][:], io["w3"][e].rearrange("(kc p) n -> p kc n", p=128), (), [B_EW3[eb]])
                k.dma("pool", EW2[eb][:], io["w2"][e].rearrange("(fc p) n -> p fc n", p=128), (), [B_EW2[eb]])
                for tt_ in range(4):
                    hi = (e * 4 + tt_) % 2
                    for fc in range(4):
                        pa = (fc % 2) * 2
                        for kc in range(8):
                            k.mm(psb[pa][:, :], EW1[eb][:, kc, fc * 128:(fc + 1) * 128], H2T[:, kc, tt_ * 512:(tt_ + 1) * 512],
                                 kc == 0, kc == 7, [B_EW1[eb]] + B_H2T[tt_ * 4:(tt_ + 1) * 4], [B_ps[pa]])
                        for kc in range(8):
                            k.mm(psb[pa + 1][:, :], EW3[eb][:, kc, fc * 128:(fc + 1) * 128], H2T[:, kc, tt_ * 512:(tt_ + 1) * 512],
                                 kc == 0, kc == 7, [B_EW3[eb]] + B_H2T[tt_ * 4:(tt_ + 1) * 4], [B_ps[pa + 1]])
                        si = fc % 2
                        k.act(SIL[si][:], psb[pa][:, :], AF.Silu, [B_ps[pa]], [B_SIL[si]])
                        k.tt(HID[hi][:, fc, :], SIL[si][:], psb[pa + 1][:, :], ALU.mult, [B_SIL[si], B_ps[pa + 1]], [B_HID[hi]])
                    for blk in range(4):
                        gb = tt_ * 4 + blk
                        for nb in range(2):
                            pb = 4 + (2 * blk + nb) % 4
                            for fc in range(4):
                                k.mm(psb[pb][:, :], HID[hi][:, fc, blk * 128:(blk + 1) * 128], EW2[eb][:, fc, nb * 512:(nb + 1) * 512],
                                     fc == 0, fc == 3, [B_HID[hi], B_EW2[eb]], [B_ps[pb]])
                            dst = ACC[:, gb, nb * 512:(nb + 1) * 512]
                            if e == 0:
                                k.ts(dst, psb[pb][:, :], GW[:, gb, e:e + 1], ALU.mult, [B_ps[pb], B_GW[gb]], [B_ACC[gb]])
                            else:
                                k.stt(dst, psb[pb][:, :], GW[:, gb, e:e + 1], dst, ALU.mult, ALU.add,
                                      [B_ps[pb], B_GW[gb], B_ACC[gb]], [B_ACC[gb]])
            for gb in range(16):
                xi = gb % 2
                k.dma("sp", xo[xi][:], io["x1d"][gb * 128:(gb + 1) * 128, :], [io["B_x1d"]], [B_xo[xi]])
                k.tt(ACC[:, gb, :], ACC[:, gb, :], ctx["GATE2"], ALU.mult, [B_ACC[gb], ctx["B_MOD"]], [B_ACC[gb]])
                k.tt(xo[xi][:], xo[xi][:], ACC[:, gb, :], ALU.add, [B_xo[xi], B_ACC[gb]], [B_xo[xi]])
                k.dma("sp", io["out"][gb * 128:(gb + 1) * 128, :], xo[xi][:], [B_xo[xi]], [io["B_out"]], dsem="out_w%d" % xi)
            S.flush(st)


def routing(ctx, rl, B_rl, rw, B_rw, gw_out, B_gw):
    k = ctx["k"]
    R, W = [B_rl, B_rw], [B_rw]
    gmax = rw[:, 0:1]
    ge = rw[:, 1:5]
    gsum = rw[:, 5:6]
    gtp = rw[:, 6:7]
    ohg = rw[:, 8:12]
    pen = rw[:, 12:16]
    msk = rw[:, 16:48]
    m1 = rw[:, 48:49]
    oh1 = rw[:, 49:81]
    msk2 = rw[:, 81:113]
    m2 = rw[:, 113:114]
    oh2 = rw[:, 114:146]
    dd = rw[:, 146:147]
    p1 = rw[:, 147:148]
    p2 = rw[:, 148:149]
    ngmax = rw[:, 149:150]
    S = ctx["S"]
    S.op("dve", lambda e: e.reduce_max(out=gmax, in_=rl[:, 0:4], axis=AX.X), [B_rl], W)
    yield
    k.ts(ngmax, gmax, -1.0, ALU.mult, R, W)
    yield
    k.act(ge, rl[:, 0:4], AF.Exp, R, W, bias=ngmax, accum_out=gsum)
    yield
    k.recip(gtp, gsum, R, W)
    yield
    k.ts(ohg, rl[:, 0:4], gmax, ALU.is_equal, R, W)
    yield
    k.ts(pen, ohg, -1.0, ALU.add, R, W, s2=-NEG, op1=ALU.mult)
    yield
    k.tt(msk.rearrange("p (g e) -> p g e", g=4), rl[:, 4:36].rearrange("p (g e) -> p g e", g=4),
         pen.unsqueeze(2).to_broadcast([128, 4, 8]), ALU.add, R, W)
    yield
    S.op("dve", lambda e: e.reduce_max(out=m1, in_=msk, axis=AX.X), R, W)
    yield
    k.ts(oh1, msk, m1, ALU.is_equal, R, W)
    yield
    k.stt(msk2, oh1, NEG, msk, ALU.mult, ALU.add, R, W)
    yield
    S.op("dve", lambda e: e.reduce_max(out=m2, in_=msk2, axis=AX.X), R, W)
    yield
    k.ts(oh2, msk2, m2, ALU.is_equal, R, W)
    yield
    k.tt(dd, m2, m1, ALU.subtract, R, W)
    yield
    k.act(dd, dd, AF.Exp, R, W)
    yield
    k.ts(p1, dd, 1.0, ALU.add, R, W)
    yield
    k.recip(p1, p1, R, W)
    yield
    k.tt(p2, dd, p1, ALU.mult, R, W)
    yield
    k.tt(p1, p1, gtp, ALU.mult, R, W)
    yield
    k.tt(p2, p2, gtp, ALU.mult, R, W)
    yield
    k.ts(oh1, oh1, p1, ALU.mult, R, W)
    yield
    k.stt(gw_out, oh2, p2, oh1, ALU.mult, ALU.add, R, [B_gw])
    yield


def phase1(ctx, io):
    nc, S, k, st, sb = ctx["nc"], ctx["S"], ctx["k"], ctx["st"], ctx["sb"]
    psb, B_ps = ctx["psb"], ctx["B_ps"]
    dbg = ctx["dbg"]
    BA, B_BA = ctx["BA"], ctx["B_BA"]
    onesf, epsc, B_const = ctx["onesf"], ctx["epsc"], ctx["B_const"]
    NT = dbg.get("p1_tiles", 16)
    with ExitStack() as s1:
        KT = [sb(s1, "KT%d" % m, [67, SEQ], BF16) for m in range(2)]
        B_KT = [[Buf("KT%d_%d" % (m, t)) for t in range(16)] for m in range(2)]
        B_KTaug = Buf("KTaug")
        QT = [sb(s1, "QT%d" % m, [67, SEQ], BF16) for m in range(2)]
        B_QT = [[Buf("QT%d_%d" % (m, t)) for t in range(16)] for m in range(2)]
        B_QTaug = Buf("QTaug")
        VA = sb(s1, "VA", [128, 64, 128], BF16)
        B_VA = [Buf("VA%d" % t) for t in range(16)]
        qkgt = sb(s1, "qkgt", [64, 3])
        onAc = sb(s1, "onAc_sb", [128, 1])
        onesb = sb(s1, "onesb", [128, 128], BF16)
        cwt = sb(s1, "cwt", [128, 12])
        B_c1 = Buf("p1const")
        k.dma("sp", qkgt[:, 0:2], io["qkg"], (), [B_c1], dsem="p1c2")
        k.dma("sp", onAc[:], io["onAc"], (), [B_c1], dsem="p1c3")
        k.dma("sp", cwt[:], io["convw"], (), [B_c1], dsem="p1c4")
        k.ts(qkgt[:, 2:3], qkgt[:, 0:1], 0.125, ALU.mult, [B_c1], [B_c1])
        k.copy(onesb[:], onesf[:], [B_const], [B_c1], eng="dve")
        k.ts(onAc[:], onAc[:], 1.0 - LAMBDA_INIT, ALU.mult, [B_c1], [B_c1])
        for m in range(2):
            k.dma("pool", KT[m][64:67, :], io["c_aug_k"], (), [B_KTaug], dsem="p1aug_k%d" % m)
            k.dma("pool", QT[m][64:67, :], io["c_aug_q"], (), [B_QTaug], dsem="p1aug_q%d" % m)

        with ExitStack() as s1a:
            WAb = sb(s1a, "WAb", [128, 8, 898], BF16)
            B_WAb = Buf("WAb")
            k.dma("pool", WAb[:], io["wA"].rearrange("(kc p) n -> p kc n", p=128), (), [B_WAb])
            xt = [sb(s1a, "xt%d" % i, [128, D]) for i in range(4)]
            B_xt = [Buf("xt%d" % i) for i in range(4)]
            ssq4 = sb(s1a, "ssq4", [128, 4, 4])
            B_ssq4 = [Buf("ssq4_%d" % i) for i in range(4)]
            junk = sb(s1a, "junk1", [128, D], BF16)
            B_junk = Buf("junk1")
            tmpf = sb(s1a, "tmpf1", [128, D])
            B_tmpf = Buf("tmpf1")
            ssq = sb(s1a, "ssq1", [128, 4])
            B_ssq = Buf("ssq1")
            hb = [sb(s1a, "hb1_%d" % i, [128, D], BF16) for i in range(2)]
            B_hb = [Buf("hb1_0"), Buf("hb1_1")]
            hT = [sb(s1a, "hT1_%d" % i, [128, 8, 512], BF16) for i in range(2)]
            B_hT = [Buf("hT1_0"), Buf("hT1_1")]
            _sq = [sb(s1a, "sq%d" % i, [128, 512]) for i in range(4)]
            sq = [_sq[0], _sq[1], None, None, _sq[2], _sq[3]]
            B_sq = [Buf("sq%d" % i) for i in range(6)]
            CONVIN = sb(s1a, "CONVIN", [128, 3, 515])
            B_cin = [Buf("cin%d" % i) for i in range(3)]
            cacc = [sb(s1a, "cacc%d" % i, [128, 512]) for i in range(3)]
            csil = [sb(s1a, "csil%d" % i, [128, 512]) for i in range(3)]
            B_cacc = [Buf("cacc%d" % i) for i in range(3)]
            B_csil = [Buf("csil%d" % i) for i in range(3)]
            gst = [sb(s1a, "gst%d" % i, [128, 3, 512], BF16) for i in range(2)]
            B_gst = [Buf("gst0"), Buf("gst1")]
            zst = [sb(s1a, "zst%d" % i, [128, 4, 128]) for i in range(2)]
            B_zst = [Buf("zst0"), Buf("zst1")]
            k.memset(CONVIN[:, :, 0:3], 0.0, B_cin, eng="pool")

            fm_groups = [("q", 0, 0, 128), ("k", 0, 128, 128), ("g", 0, 256, 128), ("g", 1, 384, 128), ("g", 2, 512, 128)]
            fm_bank = [0, 1, 2, 3, 4]
            xv = io["xb"]
            sqb = [sb(s1a, "sqb%d" % i, [128, 512], BF16) for i in range(4)]
            B_sqb = [Buf("sqb%d" % i) for i in range(4)]
            QS = [sb(s1a, "QS%d" % i, [128, 512], BF16) for i in range(4)]
            B_QS = [Buf("QS%d" % i) for i in range(4)]
            gain2 = sb(s1a, "gain2c", [128, 2])
            bd = sb(s1a, "bd64", [128, 128], BF16)
            k.memset(bd[:], 0.0, [B_c1], eng="dve")
            k.memset(bd[0:64, 0:64], 1.0, [B_c1], eng="dve")
            k.memset(bd[64:128, 64:128], 1.0, [B_c1], eng="dve")
            k.dma("sp", gain2[0:64, :], io["qkg"], (), [B_c1], dsem="p1c5")
            k.dma("sp", gain2[64:128, :], io["qkg"], (), [B_c1], dsem="p1c6")
            k.ts(gain2[:, 0:1], gain2[:, 0:1], 0.125, ALU.mult, [B_c1], [B_c1])

            def part1(t):
                hi = t % 2
                for blk in range(4):
                    gb = 4 * t + blk
                    k.dma("sp", xt[blk][:], xv[gb * 128:(gb + 1) * 128, :], (), [B_xt[blk]])
                for blk in range(4):
                    k.act(junk[:], xt[blk][:], AF.Square, [B_xt[blk]], [B_junk, B_ssq4[blk]], accum_out=ssq4[:, blk, 0:1])
                for blk in range(4):
                    k.act(ssq4[:, blk, 1:2], ssq4[:, blk, 0:1], AF.Ln, [B_ssq4[blk]], [B_ssq4[blk]], scale=1.0 / D, bias=epsc[:, 0:1])
                for blk in range(4):
                    k.act(ssq4[:, blk, 2:3], ssq4[:, blk, 1:2], AF.Exp, [B_ssq4[blk]], [B_ssq4[blk]], scale=-0.5)
                for blk in range(4):
                    hbi = blk % 2
                    k.stt(tmpf[:], xt[blk][:], ssq4[:, blk, 2:3], ctx["G1"], ALU.mult, ALU.mult, [B_xt[blk], B_ssq4[blk], ctx["B_MOD"]], [B_tmpf])
                    k.tt(hb[hbi][:], tmpf[:], ctx["SH1"], ALU.add, [B_tmpf, ctx["B_MOD"]], [B_hb[hbi]])
                    pbk = 7 if blk % 2 == 0 else 6
                    pv = psb[pbk][:, :].bitcast(BF16)
                    for kc in range(8):
                        k.tr(pv[:, kc * 128:(kc + 1) * 128], hb[hbi][:, kc * 128:(kc + 1) * 128], ctx["identb"][:], [B_hb[hbi], B_const], [B_ps[pbk]])
                    k.copy(hT[hi][:, :, blk * 128:(blk + 1) * 128], pv.rearrange("p (kc t) -> p kc t", kc=8), [B_ps[pbk]], [B_hT[hi]],
                           eng="act" if blk % 2 == 0 else "dve")

            def part2(t):
                hi = t % 2
                for gi_, (kind, idx, c0, M) in enumerate(fm_groups):
                    for kc in range(8):
                        k.mm(psb[gi_][0:M, :], WAb[:, kc, c0:c0 + M], hT[hi][:, kc, :], kc == 0, kc == 7, [B_WAb, B_hT[hi]], [B_ps[gi_]])

            def part3(t):
                par = t % 2
                for gi_ in range(2):
                    si = 2 * par + gi_
                    k.act(sqb[si][:], psb[gi_][:, :], AF.Square, [B_ps[gi_]], [B_sqb[si]])
                for gi_ in range(2, 5):
                    ch = gi_ - 2
                    k.copy(CONVIN[:, ch, 3:515], psb[gi_][:, :], [B_ps[gi_]], [B_cin[ch]], eng="dve" if ch == 0 else "act")
                for ch in range(3):
                    k.act(cacc[ch][:], CONVIN[:, ch, 0:512], AF.Identity, [B_cin[ch], B_c1], [B_cacc[ch]],
                          scale=cwt[:, ch * 4:ch * 4 + 1])
                for gi_ in range(2):
                    si = 2 * par + gi_
                    k.mm(psb[5][:, :], bd[:, :], sqb[si][:], True, True, [B_sqb[si], B_c1], [B_ps[5]])
                    k.act(sq[gi_][:], psb[5][:, :], AF.Ln, [B_ps[5]], [B_sq[gi_]], scale=1.0 / 64, bias=epsc[:, 0:1])
                for j in range(1, 4):
                    for ch in range(3):
                        k.stt(cacc[ch][:], CONVIN[:, ch, j:j + 512], cwt[:, ch * 4 + j:ch * 4 + j + 1], cacc[ch][:], ALU.mult, ALU.add,
                              [B_cin[ch], B_c1, B_cacc[ch]], [B_cacc[ch]])
                for gi_ in range(2):
                    k.act(sq[gi_][:], sq[gi_][:], AF.Exp, [B_sq[gi_]], [B_sq[gi_]], scale=-0.5)
                for ch in range(3):
                    k.copy(CONVIN[:, ch, 0:3], CONVIN[:, ch, 512:515], [B_cin[ch]], [B_cin[ch]], eng="pool")
                    k.act(csil[ch][:], cacc[ch][:], AF.Exp, [B_cacc[ch]], [B_csil[ch]], scale=-1.0)
                    k.act(csil[ch][:], csil[ch][:], AF.Ln, [B_csil[ch]], [B_csil[ch]], bias=onesf[:, 0:1])
                    k.act(csil[ch][:], csil[ch][:], AF.Exp, [B_csil[ch]], [B_csil[ch]], scale=-1.0)
                    k.tt(csil[ch][:], csil[ch][:], cacc[ch][:], ALU.mult, [B_csil[ch], B_cacc[ch]], [B_csil[ch]])
                for gi_ in range(2):
                    si = 2 * par + gi_
                    k.stt(QS[si][:], psb[gi_][:, :], gain2[:, gi_:gi_ + 1], sq[gi_][:], ALU.mult, ALU.mult,
                          [B_ps[gi_], B_sq[gi_], B_c1], [B_QS[si]])
                    dstT, B_d = (QT, B_QT) if gi_ == 0 else (KT, B_KT)
                    k.dma("pool", dstT[0][0:64, t * 512:(t + 1) * 512], QS[si][0:64, :], [B_QS[si]], [B_d[0][t]],
                          dsem="qs%d_%d_a" % (par, gi_))
                    k.dma("pool", dstT[1][0:64, t * 512:(t + 1) * 512], QS[si][64:128, :], [B_QS[si]], [B_d[1][t]],
                          dsem="qs%d_%d_b" % (par, gi_))
                gs = t % 2
                for ch in range(2):
                    si = 2 * par + ch
                    k.act(sqb[si][:], csil[ch][:], AF.Square, [B_csil[ch]], [B_sqb[si]])
                k.copy(gst[gs][:, 2, :], csil[2][:], [B_csil[2]], [B_gst[gs]], eng="act")
                for ch in range(2):
                    si = 2 * par + ch
                    k.mm(psb[6][:, :], onesb[:, :], sqb[si][:], True, True, [B_sqb[si], B_c1], [B_ps[6]])
                    k.act(sq[4 + ch][:], psb[6][:, :], AF.Ln, [B_ps[6]], [B_sq[4 + ch]], scale=1.0, bias=epsc[:, 0:1])
                for ch in range(2):
                    k.act(sq[4 + ch][:], sq[4 + ch][:], AF.Exp, [B_sq[4 + ch]], [B_sq[4 + ch]], scale=-0.5)
                    k.tt(gst[gs][:, ch, :], csil[ch][:], sq[4 + ch][:], ALU.mult, [B_csil[ch], B_sq[4 + ch]], [B_gst[gs]])
                k.dma("pool", io["gdn_d"].rearrange("c p n -> p c n")[:, :, t * 512:(t + 1) * 512], gst[gs][:],
                      [B_gst[gs]], [ctx["B_gdn"]], dsem="gdn_w%d" % gs)

            def part4(t):
                hi = t % 2
                zs = t % 2
                for blk in range(4):
                    gb = 4 * t + blk
                    pb = 7 if blk % 2 == 0 else 5
                    for kc in range(8):
                        k.mm(psb[pb][:, 0:258], hT[hi][:, kc, blk * 128:(blk + 1) * 128], WAb[:, kc, 640:898], kc == 0, kc == 7,
                             [B_hT[hi], B_WAb], [B_ps[pb]])
                    k.copy(VA[:, gb, :], psb[pb][:, 0:128], [B_ps[pb]], [B_VA[t]], eng="act")
                    k.copy(zst[zs][:, blk, :], psb[pb][:, 128:256], [B_ps[pb]], [B_zst[zs]], eng="dve")
                    k.copy(BA[:, gb, :], psb[pb][:, 256:258], [B_ps[pb]], [B_BA], eng="dve")
                k.dma("pool", io["z_d"][t * 512:(t + 1) * 512, :].rearrange("(b p) d -> p b d", p=128), zst[zs][:],
                      [B_zst[zs]], [ctx["B_zd"]], dsem="zd_w%d" % zs)

            part1(0)
            for t in range(NT):
                part2(t)
                if t + 1 < NT:
                    part1(t + 1)
                part4(t)
                part3(t)
            S.flush(st)

        if dbg.get("p1_lvl", 9) < 4:
            return
        with ExitStack() as s1b:
            biasT = sb(s1b, "biasT", [128, 64])
            dmask = sb(s1b, "dmask", [128, 4, 512])
            k.dma("sp", biasT[:], io["c_bias"], (), [B_c1], dsem="p1c0")
            k.dma("sp", dmask[:], io["c_dmask"].rearrange("p (d n) -> p d n", d=4), (), [B_c1], dsem="p1c1")
            Pt = [sb(s1b, "Pt%d" % i, [128, 512], BF16) for i in range(4)]
            B_Pt = [Buf("Pt%d" % i) for i in range(4)]
            S2 = [sb(s1b, "S2_%d" % i, [128, 512]) for i in range(2)]
            B_S2 = [Buf("S2_0"), Buf("S2_1")]
            Lacc = sb(s1b, "Lacc0", [128, 512])
            B_Lacc = Buf("Lacc0")
            Rinv = [sb(s1b, "Rinv%d" % i, [128, 512]) for i in range(2)]
            B_Rinv = [Buf("Rinv0"), Buf("Rinv1")]
            of = [sb(s1b, "of%d" % i, [128, 512]) for i in range(2)]
            B_of = [Buf("of0"), Buf("of1")]
            OAT = [sb(s1b, "OAT%d" % i, [128, 512], BF16) for i in range(2)]
            B_OAT = [Buf("OAT0"), Buf("OAT1")]
            stt_ = {"pcount": 0}

            def attention(t):
                nkb = 4 * t + 4
                steps = [(kb, m) for kb in range(nkb) for m in range(2)]
                LA = 2
                pbase = stt_["pcount"]
                stt_["pcount"] += len(steps)
                qs = slice(t * 512, (t + 1) * 512)

                def emit_s(i):
                    kb, m = steps[i]
                    d = kb - 4 * t
                    sbk = (pbase + i) % 3
                    if d < 0:
                        k.mm(psb[sbk][:, :], KT[m][0:67, kb * 128:(kb + 1) * 128], QT[m][0:67, qs], True, True,
                             [B_KT[m][kb // 4], B_KTaug, B_QT[m][t], B_QTaug], [B_ps[sbk]])
                    else:
                        k.mm(psb[sbk][:, :], KT[m][0:64, kb * 128:(kb + 1) * 128], QT[m][0:64, qs], True, True,
                             [B_KT[m][kb // 4], B_QT[m][t]], [B_ps[sbk]])

                for i in range(min(LA, len(steps))):
                    emit_s(i)
                for i, (kb, m) in enumerate(steps):
                    d = kb - 4 * t
                    sbk = (pbase + i) % 3
                    pi = (pbase + i) % 4
                    if d < 0:
                        n = 4 * t - kb
                        k.act(Pt[pi][:], psb[sbk][:, :], AF.Exp, [B_ps[sbk], B_c1], [B_Pt[pi]], bias=biasT[:, n:n + 1])
                    else:
                        s2i = (pbase + i) % 2
                        k.tt(S2[s2i][:], psb[sbk][:, :], dmask[:, d, :], ALU.add, [B_ps[sbk], B_c1], [B_S2[s2i]])
                        k.act(Pt[pi][:], S2[s2i][:], AF.Exp, [B_S2[s2i]], [B_Pt[pi]])
                    if i + LA < len(steps):
                        emit_s(i + LA)
                    k.mm(psb[3 + m][:, :], VA[:, kb, :], Pt[pi][:], kb == 0, kb == nkb - 1, [B_Pt[pi], B_VA[kb // 4]],
                         [B_ps[3 + m]])
                    k.mm(psb[5 + m][:, :], onesb[:, :], Pt[pi][:], kb == 0, kb == nkb - 1, [B_Pt[pi], B_c1], [B_ps[5 + m]])
                    yield

            def finalize(t):
                oi = t % 2
                for m in range(2):
                    k.recip(Rinv[m][:], psb[5 + m][:, :], [B_ps[5 + m]], [B_Rinv[m]])
                k.tt(of[0][:], psb[3][:, :], Rinv[0][:], ALU.mult, [B_ps[3], B_Rinv[0]], [B_of[0]])
                k.tt(of[1][:], psb[4][:, :], Rinv[1][:], ALU.mult, [B_ps[4], B_Rinv[1]], [B_of[1]])
                yield
                k.stt(of[0][:], of[1][:], ctx["neglam"][:, 0:1], of[0][:], ALU.mult, ALU.add, [B_of[0], B_of[1], ctx["B_neglam"]], [B_of[0]])
                yield
                k.tt(of[1][:], of[0][:], of[0][:], ALU.mult, [B_of[0]], [B_of[1]])
                yield
                k.mm(psb[7][:, :], onesf[:, :], of[1][:], True, True, [B_of[1], B_const], [B_ps[7]])
                yield
                k.act(Rinv[0][:], psb[7][:, :], AF.Ln, [B_ps[7]], [B_Rinv[0]], scale=1.0 / 128, bias=epsc[:, 0:1])
                yield
                k.act(Rinv[0][:], Rinv[0][:], AF.Exp, [B_Rinv[0]], [B_Rinv[0]], scale=-0.5)
                yield
                k.stt(OAT[oi][:], of[0][:], onAc[:, 0:1], Rinv[0][:], ALU.mult, ALU.mult, [B_of[0], B_Rinv[0], B_c1], [B_OAT[oi]])
                k.dma("sp", io["exA_in"].ap()[t // 4, :, (t % 4) * 512:(t % 4 + 1) * 512], OAT[oi][:], [B_OAT[oi]],
                      [io["B_exA_in"][t // 4]], dsem="exin_a%d" % oi)
                if t % 4 == 3 and ctx.get("gatherA") is not None:
                    ctx["gatherA"](t // 4)
                yield

            fin = None
            for t in range(NT):
                for i_, _ in enumerate(attention(t)):
                    if fin is not None and i_ >= 1 and i_ % 2 == 1:
                        if next(fin, "done") == "done":
                            fin = None
                if fin is not None:
                    for _ in fin:
                        pass
                fin = finalize(t)
                next(fin)
            for _ in fin:
                pass
            S.flush(st)


def phase2(ctx, io):
    nc, S, k, st, sb = ctx["nc"], ctx["S"], ctx["k"], ctx["st"], ctx["sb"]
    psb, B_ps = ctx["psb"], ctx["B_ps"]
    dbg = ctx["dbg"]
    BA, B_BA = ctx["BA"], ctx["B_BA"]
    onesf, identf, identb, epsc, B_const = ctx["onesf"], ctx["identf"], ctx["identb"], ctx["epsc"], ctx["B_const"]
    NBLK = dbg.get("p2_blocks", 64)
    NG = NBLK // 4
    with ExitStack() as s2:
        gm = sb(s2, "gm", [128, 7, 128])
        TRI, SAMEC, SELA, SELB, MASKL, STRICT, MASKU = [gm[:, i, :] for i in range(7)]

        def bc_g(ap2):
            return ap2.unsqueeze(1).to_broadcast([128, 4, 128])

        def bc_c(ap_cols):
            return ap_cols.unsqueeze(2).to_broadcast([128, 4, 128])

        adt = sb(s2, "adt_sb", [128, 2])
        negA = sb(s2, "negA", [128, 1])
        onBt = sb(s2, "onBt", [128, 128])
        B_c2 = Buf("p2const")
        BETA = sb(s2, "BETA", [128, 64])
        Gt = sb(s2, "Gt", [128, 64])
        GC = sb(s2, "GC", [128, 64])
        GL = sb(s2, "GL", [128, 64])
        EG = sb(s2, "EG", [128, 64])
        EGL = sb(s2, "EGL", [128, 64])
        BG = sb(s2, "BG", [128, 64])
        NGC = sb(s2, "NGC", [128, 64])
        NBETA = sb(s2, "NBETA", [128, 64])
        GLB = sb(s2, "GLB", [128, 64, 2])
        B_bulk = Buf("p2bulk")
        Sst = sb(s2, "Sst", [128, 128])
        B_S = Buf("Sst")

        def mk(name, shape=(128, 4, 128), dt=F32, n=2):
            return [sb(s2, "%s%d" % (name, i), list(shape), dt) for i in range(n)], [Buf("%s%d" % (name, i)) for i in range(n)]
        qkv, B_qkv = mk("qkv", (128, 3, 512), BF16)
        KBG, B_KBG = mk("KBG")
        KDEC0, B_KDEC0 = mk("KDEC0")
        KDEC1, B_KDEC1 = mk("KDEC1")
        VB, B_VB = mk("VB")
        dG, B_dG = mk("dG", n=1)
        ER, B_ER = mk("ER", n=1)
        tD, B_tD = mk("tD", n=1)
        Dm, B_Dm = mk("Dm", n=1)
        tDT, B_tDT = mk("tDT", n=1)
        DT, B_DT = mk("DT", n=1)
        t3, B_t3 = mk("t3", n=1)
        Pa, B_Pa = mk("Pa", n=1)
        Pta, B_Pta = mk("Pta", n=1)
        Pb, B_Pb = mk("Pb", n=1)
        Ptb, B_Ptb = mk("Ptb", n=1)
        Tt, B_Tt = mk("Tt", n=1)
        QK0, B_QK0 = mk("QK0")
        QK1, B_QK1 = mk("QK1")
        QD0, B_QD0 = mk("QD0")
        QD1, B_QD1 = mk("QD1")
        WT0, B_WT0 = mk("WT0")
        WT1, B_WT1 = mk("WT1")
        U, B_U = mk("U")
        VN, B_VN = mk("VN", (128, 128), F32, 4)
        zt, B_zt = mk("zt")
        sz, B_sz = mk("sz", n=1)
        osb, B_osb = mk("osb2", n=1)
        ot, B_ot = mk("ot2", n=1)
        onb, B_onb = mk("onb2", (128, 4, 128), BF16, 1)
        ojk = sb(s2, "ojk2", [128, 128], BF16)
        B_ojk = Buf("ojk2")
        rr, B_rr = mk("rr2", (128, 12), F32, 1)
        OBT, B_OBT = mk("OBT", (128, 512), BF16)

        k.dma("sp", gm[:], io["c_gmask"].rearrange("p (a n) -> p a n", a=7), (), [B_c2], dsem="p2c0")
        k.dma("sp", adt[:], io["adt"].partition_broadcast(128), (), [B_c2], dsem="p2c1")
        k.dma("sp", onBt[:], io["onB"].partition_broadcast(128), (), [B_c2], dsem="p2c2")
        k.act(negA[:], adt[:, 0:1], AF.Exp, [B_c2], [B_c2])
        k.ts(negA[:], negA[:], -1.0, ALU.mult, [B_c2], [B_c2])
        for lst in (KDEC0, KDEC1, QK0, QK1, QD0, QD1, WT0, WT1):
            for tl in lst:
                k.memset(tl[:], 0.0, [B_c2], eng="dve")
        k.memset(Sst[:], 0.0, [B_S], eng="dve")
        RB, WB_ = [B_bulk, B_c2], [B_bulk]
        k.act(BETA[:], BA[:, :, 0], AF.Sigmoid, [B_BA], WB_)
        k.act(Gt[:], BA[:, :, 1], AF.Exp, [B_BA, B_c2], WB_, bias=adt[:, 1:2])
        k.act(Gt[:], Gt[:], AF.Ln, RB, WB_, bias=onesf[:, 0:1])
        k.ts(Gt[:], Gt[:], negA[:, 0:1], ALU.mult, RB, WB_)
        k.mm(psb[0][:, 0:64], TRI, Gt[:], True, True, RB, [B_ps[0]])
        k.mm(psb[1][:, 0:64], SAMEC, Gt[:], True, True, RB, [B_ps[1]])
        k.copy(GC[:], psb[0][:, 0:64], [B_ps[0]], WB_, eng="dve")
        k.copy(GL[:], psb[1][:, 0:64], [B_ps[1]], WB_, eng="dve")
        k.act(EG[:], GC[:], AF.Exp, RB, WB_)
        k.tt(EGL[:], GL[:], GC[:], ALU.subtract, RB, WB_)
        k.act(EGL[:], EGL[:], AF.Exp, RB, WB_)
        k.tt(BG[:], BETA[:], EG[:], ALU.mult, RB, WB_)
        k.ts(NGC[:], GC[:], -1.0, ALU.mult, RB, WB_)
        k.ts(NBETA[:], BETA[:], -1.0, ALU.mult, RB, WB_)
        k.mm(psb[2][:, 0:64], SELA, GL[:], True, True, RB, [B_ps[2]])
        k.mm(psb[3][:, 0:64], SELB, GL[:], True, True, RB, [B_ps[3]])
        k.act(GLB[:, :, 0], psb[2][:, 0:64], AF.Exp, [B_ps[2]], WB_)
        k.act(GLB[:, :, 1], psb[3][:, 0:64], AF.Exp, [B_ps[3]], WB_)

        gv = io["gdn_d"].rearrange("c p n -> p c n")

        def v4(t_):
            return t_.rearrange("p (g n) -> p g n", g=4)

        def pre(G):
            gp = G % 2
            n0 = 4 * G
            cs = slice(n0, n0 + 4)
            k.dma("sp", qkv[gp][:], gv[:, :, n0 * 128:(n0 + 4) * 128], [ctx["B_gdn"]], [B_qkv[gp]])
            k.dma("sp", zt[gp][:], io["z_d"][n0 * 128:(n0 + 4) * 128, :].rearrange("(g p) d -> p g d", p=128),
                  [ctx["B_zd"]], [B_zt[gp]])
            qT4 = v4(qkv[gp][:, 0, :])
            pv0 = psb[0][:, :].bitcast(BF16)
            for g in range(4):
                k.tr(pv0[:, g * 128:(g + 1) * 128], qkv[gp][:, 1, g * 128:(g + 1) * 128], identb[:], [B_qkv[gp], B_const], [B_ps[0]])
            for g in range(4):
                k.tr(pv0[:, 512 + g * 128:512 + (g + 1) * 128], qkv[gp][:, 2, g * 128:(g + 1) * 128], identb[:],
                     [B_qkv[gp], B_const], [B_ps[0]])
            k.tt(dG[0][:], bc_g(identf[:]), bc_c(GC[:, cs]), ALU.mult, [B_const, B_bulk], [B_dG[0]])
            yield
            k.mm(psb[1][:, :], onesf[:], dG[0][:].rearrange("p g n -> p (g n)"), True, True, [B_dG[0], B_const], [B_ps[1]])
            ktok = v4(pv0[:, 0:512])
            vtok = v4(pv0[:, 512:1024])
            k.tt(KBG[gp][:], ktok, bc_c(BG[:, cs]), ALU.mult, [B_ps[0], B_bulk], [B_KBG[gp]])
            k.tt(KDEC0[gp][0:64, :, :], ktok[0:64], bc_c(EGL[:, cs])[0:64], ALU.mult, [B_ps[0], B_bulk], [B_KDEC0[gp]])
            k.tt(KDEC1[gp][64:128, :, :], ktok[64:128], bc_c(EGL[:, cs])[64:128], ALU.mult, [B_ps[0], B_bulk], [B_KDEC1[gp]])
            k.tt(VB[gp][:], vtok, bc_c(BETA[:, cs]), ALU.mult, [B_ps[0], B_bulk], [B_VB[gp]])
            yield
            R4 = v4(psb[1][:, :])
            k.act(ER[0][:], R4, AF.Exp, [B_ps[1]], [B_ER[0]])
            k.stt(tD[0][:], R4, -1.0, bc_g(MASKL), ALU.mult, ALU.add, [B_ps[1], B_c2], [B_tD[0]])
            k.tt(tDT[0][:], R4, bc_g(MASKU), ALU.add, [B_ps[1], B_c2], [B_tDT[0]])
            for g in range(4):
                kTg = qkv[gp][:, 1, g * 128:(g + 1) * 128]
                k.mm(psb[2][:, g * 128:(g + 1) * 128], kTg, kTg, True, True, [B_qkv[gp]], [B_ps[2]])
            for g in range(4):
                kTg = qkv[gp][:, 1, g * 128:(g + 1) * 128]
                qTg = qkv[gp][:, 0, g * 128:(g + 1) * 128]
                k.mm(psb[3][:, g * 128:(g + 1) * 128], kTg, qTg, True, True, [B_qkv[gp]], [B_ps[3]])
            yield
            k.tt(QD0[gp][:, :, 0:64], qT4[:, :, 0:64], ER[0][:, :, 0:64], ALU.mult, [B_qkv[gp], B_ER[0]], [B_QD0[gp]])
            k.tt(QD1[gp][:, :, 64:128], qT4[:, :, 64:128], ER[0][:, :, 64:128], ALU.mult, [B_qkv[gp], B_ER[0]], [B_QD1[gp]])
            yield
            for g in range(4):
                k.act(Dm[0][:, g, :], tD[0][:, g, :], AF.Exp, [B_tD[0], B_bulk], [B_Dm[0]], bias=GC[:, n0 + g:n0 + g + 1])
            for g in range(4):
                k.act(DT[0][:, g, :], tDT[0][:, g, :], AF.Exp, [B_tDT[0], B_bulk], [B_DT[0]], bias=NGC[:, n0 + g:n0 + g + 1])
            yield
            k.tt(t3[0][:], v4(psb[2][:, :]), Dm[0][:], ALU.mult, [B_ps[2], B_Dm[0]], [B_t3[0]])
            k.tt(t3[0][:], t3[0][:], bc_c(NBETA[:, cs]), ALU.mult, [B_t3[0], B_bulk], [B_t3[0]])
            k.tt(Pa[0][:], t3[0][:], bc_g(STRICT), ALU.mult, [B_t3[0], B_c2], [B_Pa[0]])
            qk4 = v4(psb[3][:, :])
            k.tt(QK0[gp][:, :, 0:64], qk4[:, :, 0:64], DT[0][:, :, 0:64], ALU.mult, [B_ps[3], B_DT[0]], [B_QK0[gp]])
            k.tt(QK1[gp][:, :, 64:128], qk4[:, :, 64:128], DT[0][:, :, 64:128], ALU.mult, [B_ps[3], B_DT[0]], [B_QK1[gp]])
            yield
            for g in range(4):
                k.tr(psb[4][:, g * 128:(g + 1) * 128], Pa[0][:, g, :], identf[:], [B_Pa[0], B_const], [B_ps[4]])
            yield
            k.copy(Pta[0][:], v4(psb[4][:, :]), [B_ps[4]], [B_Pta[0]], eng="act")
            k.tt(Tt[0][:], Pta[0][:], bc_g(identf[:]), ALU.add, [B_Pta[0], B_const], [B_Tt[0]])
            yield
            P, BP, PT, BPT = Pa[0], B_Pa[0], Pta[0], B_Pta[0]
            Pn, BPn, PTn, BPTn = Pb[0], B_Pb[0], Ptb[0], B_Ptb[0]
            for lvl in range(1, 6):
                for g in range(4):
                    k.mm(psb[2][:, g * 128:(g + 1) * 128], PT[:, g, :], P[:, g, :], True, True, [BP, BPT], [B_ps[2]])
                if lvl < 5:
                    for g in range(4):
                        k.mm(psb[3][:, g * 128:(g + 1) * 128], P[:, g, :], PT[:, g, :], True, True, [BP, BPT], [B_ps[3]])
                yield
                k.copy(Pn[:], v4(psb[2][:, :]), [B_ps[2]], [BPn], eng="act")
                if lvl < 5:
                    k.copy(PTn[:], v4(psb[3][:, :]), [B_ps[3]], [BPTn], eng="dve")
                yield
                for g in range(4):
                    k.mm(psb[4][:, g * 128:(g + 1) * 128], Pn[:, g, :], Tt[0][:, g, :], True, True, [BPn, B_Tt[0]], [B_ps[4]])
                yield
                k.tt(Tt[0][:], Tt[0][:], v4(psb[4][:, :]), ALU.add, [B_Tt[0], B_ps[4]], [B_Tt[0]])
                yield
                P, BP, PT, BPT, Pn, BPn, PTn, BPTn = Pn, BPn, PTn, BPTn, P, BP, PT, BPT
            for g in range(4):
                k.mm(psb[2][:, g * 128:(g + 1) * 128], Tt[0][:, g, :], VB[gp][:, g, :], True, True, [B_Tt[0], B_VB[gp]], [B_ps[2]])
            for g in range(4):
                k.mm(psb[3][:, g * 128:(g + 1) * 128], KBG[gp][:, g, :], Tt[0][:, g, :], True, True, [B_Tt[0], B_KBG[gp]], [B_ps[3]])
            yield
            k.copy(U[gp][:], v4(psb[2][:, :]), [B_ps[2]], [B_U[gp]], eng="act")
            w4 = v4(psb[3][:, :])
            k.copy(WT0[gp][:, :, 0:64], w4[:, :, 0:64], [B_ps[3]], [B_WT0[gp]], eng="dve")
            k.copy(WT1[gp][:, :, 64:128], w4[:, :, 64:128], [B_ps[3]], [B_WT1[gp]], eng="dve")
            yield

        vn_state = {"i": 0}

        def scan(G):
            gp = G % 2
            n0 = 4 * G
            for g in range(4):
                n = n0 + g
                for c in range(2):
                    WTc, BWTc = (WT0[gp], B_WT0[gp]) if c == 0 else (WT1[gp], B_WT1[gp])
                    QDc, BQDc = (QD0[gp], B_QD0[gp]) if c == 0 else (QD1[gp], B_QD1[gp])
                    QKc, BQKc = (QK0[gp], B_QK0[gp]) if c == 0 else (QK1[gp], B_QK1[gp])
                    KDc, BKDc = (KDEC0[gp], B_KDEC0[gp]) if c == 0 else (KDEC1[gp], B_KDEC1[gp])
                    vi = vn_state["i"] % 4
                    vn_state["i"] += 1
                    p1 = psb[5][:, c * 128:(c + 1) * 128]
                    p2 = psb[5][:, 256 + c * 128:256 + (c + 1) * 128]
                    og = psb[6][:, g * 128:(g + 1) * 128]
                    k.mm(p1, WTc[:, g, :], Sst[:], True, True, [BWTc, B_S], [B_ps[5]])
                    k.mm(og, QDc[:, g, :], Sst[:], c == 0, False, [BQDc, B_S], [B_ps[6]], sgc=True)
                    yield
                    k.tt(VN[vi][:], U[gp][:, g, :], p1, ALU.subtract, [B_U[gp], B_ps[5]], [B_VN[vi]])
                    yield
                    k.mm(p2, KDc[:, g, :], VN[vi][:], True, True, [BKDc, B_VN[vi]], [B_ps[5]])
                    k.mm(og, QKc[:, g, :], VN[vi][:], False, c == 1, [BQKc, B_VN[vi]], [B_ps[6]], sgc=True)
                    yield
                    k.stt(Sst[:], Sst[:], GLB[:, n, c:c + 1], p2, ALU.mult, ALU.add, [B_S, B_bulk, B_ps[5]], [B_S])
                    yield
            r = rr[0]
            R_, W_ = [B_rr[0]], [B_rr[0]]
            k.act(osb[0][:], v4(psb[6][:, :]), AF.Identity, [B_ps[6]], [B_osb[0]], scale=128.0 ** -0.5)
            k.act(sz[0][:], zt[gp][:], AF.Exp, [B_zt[gp]], [B_sz[0]], scale=-1.0)
            k.act(sz[0][:], sz[0][:], AF.Ln, [B_sz[0]], [B_sz[0]], bias=onesf[:, 0:1])
            k.act(sz[0][:], sz[0][:], AF.Exp, [B_sz[0]], [B_sz[0]], scale=-1.0)
            k.tt(sz[0][:], sz[0][:], zt[gp][:], ALU.mult, [B_sz[0], B_zt[gp]], [B_sz[0]])
            yield
            for g in range(4):
                k.act(ojk[:], osb[0][:, g, :], AF.Square, [B_osb[0]], [B_ojk] + W_, accum_out=r[:, g:g + 1])
            k.act(r[:, 4:8], r[:, 0:4], AF.Ln, R_, W_, scale=1.0 / 128, bias=epsc[:, 0:1])
            k.act(r[:, 8:12], r[:, 4:8], AF.Exp, R_, W_, scale=-0.5)
            yield
            k.tt(ot[0][:], osb[0][:], bc_c(r[:, 8:12]), ALU.mult, [B_osb[0]] + R_, [B_ot[0]])
            k.tt(ot[0][:], ot[0][:], bc_g(onBt[:]), ALU.mult, [B_ot[0], B_c2], [B_ot[0]])
            k.tt(onb[0][:], ot[0][:], sz[0][:], ALU.mult, [B_ot[0], B_sz[0]], [B_onb[0]])
            yield
            pv7 = psb[7][:, :].bitcast(BF16)
            for g in range(4):
                k.tr(pv7[:, g * 128:(g + 1) * 128], onb[0][:, g, :], identb[:], [B_onb[0], B_const], [B_ps[7]])
            yield
            oi = G % 2
            k.copy(OBT[oi][:], pv7[:, 0:512], [B_ps[7]], [B_OBT[oi]], eng="act")
            k.dma("sp", io["exB_in"].ap()[G // 4, :, (G % 4) * 512:(G % 4 + 1) * 512], OBT[oi][:], [B_OBT[oi]],
                  [io["B_exB_in"][G // 4]], dsem="exin_b%d" % oi)
            if G % 4 == 3 and ctx.get("gatherB") is not None:
                ctx["gatherB"](G // 4)
            yield

        for _ in pre(0):
            pass
        for G in range(NG):
            nxt = pre(G + 1) if G + 1 < NG else None
            for i_, _ in enumerate(scan(G)):
                if nxt is not None and i_ % 1 == 0:
                    next(nxt, None)
            if nxt is not None:
                for _ in nxt:
                    pass
        S.flush(st)


def make_consts(h):
    slope = 2.0 ** (-2.0 * (h + 1))
    c = {}
    c["c_ident"] = np.eye(128, dtype=np.float32)
    jq = np.arange(512)
    c["c_aug_q"] = np.tile(np.stack([-slope * (jq // 256 * 256), -slope * (jq % 256), np.ones(512)]), (1, 16)).astype(np.float32)
    ik = np.arange(128)
    c["c_aug_k"] = np.tile(np.stack([np.ones(128), np.ones(128), slope * ik]), (1, 64)).astype(np.float32)
    c["c_bias"] = np.tile((-slope * 128.0 * np.arange(64))[None, :], (128, 1)).astype(np.float32)
    dm = np.zeros((128, 4, 512), np.float32)
    for d in range(4):
        kp = d * 128 + ik[:, None]
        qp = jq[None, :]
        ok = (kp // 64) <= (qp // 64)
        dm[:, d, :] = np.where(ok, -slope * np.abs(qp - kp), NEG)
    c["c_dmask"] = dm.reshape(128, 2048)
    a = np.arange(128)
    same = (a[:, None] // 64) == (a[None, :] // 64)
    tri = ((a[:, None] <= a[None, :]) & same).astype(np.float32)
    samec = same.astype(np.float32)
    sela = np.zeros((128, 128), np.float32)
    sela[0, :] = 1.0
    selb = np.zeros((128, 128), np.float32)
    selb[64, :] = 1.0
    maskl = np.where((a[:, None] >= a[None, :]) & same, 0.0, NEG).astype(np.float32)
    strictl = ((a[:, None] > a[None, :]) & same).astype(np.float32)
    c["c_gmask"] = np.concatenate([tri, samec, sela, selb, maskl, strictl, np.ascontiguousarray(maskl.T)], axis=1)
    return c


def prep_inputs(inp):
    f = lambda a: np.ascontiguousarray(np.asarray(a, dtype=np.float32))
    x = f(inp["x"])
    w_in = f(inp["w_in"])[0]
    shared = {
        "w_ada": f(inp["w_ada"])[0], "b_ada": f(inp["b_ada"])[0][None, :],
        "gain1": f(inp["norm1_gain"]), "gain2": f(inp["norm2_gain"]),
        "wG": np.ascontiguousarray(w_in[:, 3592:]),
        "qkg": np.ascontiguousarray(np.stack([f(inp["da_q_norm"])[0], f(inp["da_k_norm"])[0]], axis=1)),
        "lamv": np.ascontiguousarray(np.stack([f(inp["da_lambda_q1"])[0], f(inp["da_lambda_k1"])[0],
                                               f(inp["da_lambda_q2"])[0], f(inp["da_lambda_k2"])[0]], axis=1)),
        "onAc": np.ascontiguousarray(f(inp["da_out_norm"]).T), "onB": f(inp["gdn_out_norm"]),
        "w_ba": f(inp["w_branch_a"])[0], "w_bb": f(inp["w_branch_b"])[0], "w_out": f(inp["w_out"])[0],
        "w_rt": np.ascontiguousarray(np.concatenate([f(inp["w_group"])[0], f(inp["w_router"])[0]], axis=1)),
        "b_rt": np.ascontiguousarray(np.concatenate([f(inp["b_group"])[0], f(inp["b_router"])[0]])[None, :]),
        "w1": f(inp["w1"])[0], "w3": f(inp["w3"])[0], "w2": f(inp["w2"])[0],
    }
    conv = f(inp["gdn_conv"])[0]
    maps = []
    for c in range(8):
        b, h = c // 4, c % 4
        cols = np.concatenate([
            h * 128 + np.arange(128), 512 + h * 128 + np.arange(128),
            1536 + h * 128 + np.arange(128), 2048 + h * 128 + np.arange(128), 2560 + h * 128 + np.arange(128),
            1024 + h * 128 + np.arange(128), 3072 + h * 128 + np.arange(128),
            np.array([3584 + h, 3588 + h])])
        m = dict(shared)
        m["xb"] = x[b]
        m["xs"] = np.ascontiguousarray(x[b, h * NTOK_C:(h + 1) * NTOK_C])
        m["cT"] = np.ascontiguousarray(f(inp["c"])[b].reshape(8, 128).T)
        m["wA"] = np.ascontiguousarray(w_in[:, cols])
        m["convw"] = np.ascontiguousarray(
            np.stack([conv[:, t * 512 + h * 128: t * 512 + (h + 1) * 128] for t in range(3)], axis=0)
            .transpose(2, 0, 1).reshape(128, 12))
        m["adt"] = np.array([[f(inp["gdn_a_log"])[0, h], f(inp["gdn_dt_bias"])[0, h]]], np.float32)
        m.update(make_consts(h))
        maps.append(m)
    return maps


_PROG = None


def kernel(**inputs):
    global _PROG
    if _PROG is None:
        _PROG = build_program()
    nc, _ = _PROG
    maps = prep_inputs(inputs)
    res = run_bass_kernel_spmd(nc, maps, core_ids=list(range(8)))
    out = np.empty((2, SEQ, D), np.float32)
    for c in range(8):
        b, j = c // 4, c % 4
        out[b, j * NTOK_C:(j + 1) * NTOK_C] = res.results[c]["out"]
    return out
```

```python
import math
from contextlib import ExitStack

import numpy as np
import ml_dtypes

import concourse.bass as bass
import concourse.mybir as mybir
from concourse.bass_utils import run_bass_kernel_spmd

F32 = mybir.dt.float32
BF16 = mybir.dt.bfloat16
I32 = mybir.dt.int32
AF = mybir.ActivationFunctionType
ALU = mybir.AluOpType
AX = mybir.AxisListType

D = 1024
SEQ = 8192
NTOK_C = 2048
EPS = 1e-6
NEG = -30000.0
LAMBDA_INIT = 0.8 - 0.6 * math.exp(-0.3 * 0)
NEXP = 32
DEXP = 512

ENGS = ("pe", "act", "dve", "pool", "sp")


class Buf:
    __slots__ = ("name", "last_w", "readers", "excl")

    def __init__(self, name, excl=False):
        self.name = name
        self.last_w = None
        self.readers = []
        self.excl = excl


class Op:
    __slots__ = ("eng", "fn", "deps", "is_dma", "is_cc", "dsem", "needs_inc", "val", "flushed")

    def __init__(self, eng, fn, is_dma=False, dsem=None, is_cc=False):
        self.eng = eng
        self.fn = fn
        self.deps = []
        self.is_dma = is_dma
        self.is_cc = is_cc
        self.dsem = dsem
        self.needs_inc = is_dma
        self.val = None
        self.flushed = False


class Sched:
    def __init__(self, nc):
        self.nc = nc
        self.ops = {e: [] for e in ENGS}
        self.dsem_count = {}
        self.last_on_eng = {e: None for e in ENGS}
        self.last_dma = {}
        self.pending_barrier = {e: [] for e in ENGS}
        self.nops = 0

    def _add_deps(self, op, reads, writes):
        deps = []
        xr = [b for b in reads if b.excl]
        if xr:
            reads = [b for b in reads if not b.excl]
            writes = list(writes) + [b for b in xr if b not in writes]
        for b in reads:
            if b.last_w is not None:
                deps.append(b.last_w)
        for b in writes:
            if b.last_w is not None:
                deps.append(b.last_w)
            deps.extend(b.readers)
        deps.extend(self.pending_barrier[op.eng])
        self.pending_barrier[op.eng] = []
        seen = set()
        for d in deps:
            if d is op or id(d) in seen:
                continue
            seen.add(id(d))
            if (not d.is_dma) and (not op.is_dma) and d.eng == "pe" and op.eng == "pe":
                continue
            if d.flushed and d.val is None:
                continue
            op.deps.append(d)
            d.needs_inc = True
        for b in reads:
            b.readers.append(op)
        for b in writes:
            b.last_w = op
            b.readers = []

    def op(self, eng, fn, reads=(), writes=()):
        o = Op(eng, fn)
        self._add_deps(o, reads, writes)
        self.ops[eng].append(o)
        self.last_on_eng[eng] = o
        self.nops += 1
        return o

    def dma(self, queue, pairs, reads=(), writes=(), dsem=None):
        if dsem is None:
            dsem = writes[0].name if writes else reads[0].name
        o = Op(queue, pairs, is_dma=True, dsem=dsem)
        self._add_deps(o, reads, writes)
        self.dsem_count[dsem] = self.dsem_count.get(dsem, 0) + 16 * len(pairs)
        o.val = self.dsem_count[dsem]
        self.ops[queue].append(o)
        self.last_dma[dsem] = o
        self.nops += 1
        return o

    def cc(self, fn, reads=(), writes=(), dsem="cc"):
        o = Op("pool", fn, is_dma=True, dsem=dsem, is_cc=True)
        self._add_deps(o, reads, writes)
        assert dsem not in self.dsem_count
        self.dsem_count[dsem] = 1
        o.val = 1
        self.ops["pool"].append(o)
        self.last_dma[dsem] = o
        return o

    def barrier(self):
        targets = [o for o in self.last_on_eng.values() if o is not None and not o.is_dma]
        targets += list(self.last_dma.values())
        for e in ENGS:
            self.pending_barrier[e] = list(targets)

    def flush(self, stack, final=False, final_waits_engine="sp"):
        nc = self.nc
        self.barrier()
        fin = self.pending_barrier[final_waits_engine] if final else []
        for e in ENGS:
            for d in self.pending_barrier[e]:
                d.needs_inc = True
        if not hasattr(self, "esem"):
            self.esem = {e: stack.enter_context(nc.semaphore("s_" + e)) for e in ENGS}
            self.dsem = {}
            self.ecount = {e: 0 for e in ENGS}
            self.waited = {e: {} for e in ENGS}
        for e in ENGS:
            c = self.ecount[e]
            for o in self.ops[e]:
                if not o.is_dma and o.needs_inc:
                    c += 1
                    o.val = c
            self.ecount[e] = c
        for k in self.dsem_count:
            if k not in self.dsem:
                self.dsem[k] = stack.enter_context(nc.semaphore("d_%d" % len(self.dsem)))
        esem, dsem = self.esem, self.dsem

        def sem_of(d):
            return (dsem[d.dsem], d.val) if d.is_dma else (esem[d.eng], d.val)

        ops = self.ops
        self.ops = {e: [] for e in ENGS}
        with nc.Block() as block:

            def run(engname, eng):
                waited = self.waited[engname]
                for o in ops[engname]:
                    for d in o.deps:
                        s, v = sem_of(d)
                        key = id(s)
                        if waited.get(key, 0) >= v:
                            continue
                        waited[key] = v
                        eng.wait_ge(s, v)
                    if o.is_cc:
                        o.fn(eng).then_inc(dsem[o.dsem])
                    elif o.is_dma:
                        for (out_ap, in_ap) in o.fn:
                            if callable(out_ap):
                                out_ap = out_ap(eng)
                            if callable(in_ap):
                                in_ap = in_ap(eng)
                            eng.dma_start(out=out_ap, in_=in_ap).then_inc(dsem[o.dsem], 16)
                    else:
                        inst = o.fn(eng)
                        if o.needs_inc:
                            inst.then_inc(esem[engname], 1)
                    o.fn = None
                    o.flushed = True
                if final and engname == final_waits_engine:
                    for d in fin:
                        s, v = sem_of(d)
                        if waited.get(id(s), 0) >= v:
                            continue
                        waited[id(s)] = v
                        eng.wait_ge(s, v)

            @block.sync
            def _(eng):
                run("sp", eng)

            @block.tensor
            def _(eng):
                run("pe", eng)

            @block.scalar
            def _(eng):
                run("act", eng)

            @block.vector
            def _(eng):
                run("dve", eng)

            @block.gpsimd
            def _(eng):
                run("pool", eng)


class K:
    def __init__(self, nc, S):
        self.nc = nc
        self.S = S

    def mm(self, out, lhsT, rhs, start, stop, reads, writes, sgc=False):
        if sgc:
            self.S.op("pe", lambda e: e.matmul(out, lhsT=lhsT, rhs=rhs, start=start, stop=stop, skip_group_check=True),
                      reads, writes)
        else:
            self.S.op("pe", lambda e: e.matmul(out, lhsT=lhsT, rhs=rhs, start=start, stop=stop), reads, writes)

    def tr(self, out, in_, ident, reads, writes):
        self.S.op("pe", lambda e: e.transpose(out, in_, ident), reads, writes)

    def act(self, out, in_, func, reads, writes, scale=None, bias=None, accum_out=None, eng="act"):
        kw = {}
        if scale is not None:
            kw["scale"] = scale
        if bias is not None:
            kw["bias"] = bias
        if accum_out is not None:
            kw["accum_out"] = accum_out
        self.S.op(eng, lambda e: e.activation(out=out, in_=in_, func=func, **kw), reads, writes)

    def tt(self, out, in0, in1, op, reads, writes, eng="dve"):
        self.S.op(eng, lambda e: e.tensor_tensor(out=out, in0=in0, in1=in1, op=op), reads, writes)

    def ts(self, out, in0, s1, op0, reads, writes, s2=None, op1=None, eng="dve", accum_out=None):
        kw = {}
        if op1 is not None:
            kw["op1"] = op1
        if accum_out is not None:
            kw["accum_out"] = accum_out
        self.S.op(eng, lambda e: e.tensor_scalar(out=out, in0=in0, scalar1=s1, scalar2=s2, op0=op0, **kw), reads, writes)

    def stt(self, out, in0, scalar, in1, op0, op1, reads, writes):
        self.S.op("dve", lambda e: e.scalar_tensor_tensor(out=out, in0=in0, scalar=scalar, in1=in1, op0=op0, op1=op1),
                  reads, writes)

    def copy(self, out, in_, reads, writes, eng="dve"):
        if eng == "act":
            self.S.op("act", lambda e: e.copy(out=out, in_=in_), reads, writes)
        else:
            self.S.op(eng, lambda e: e.tensor_copy(out=out, in_=in_), reads, writes)

    def recip(self, out, in_, reads, writes):
        self.S.op("dve", lambda e: e.reciprocal(out=out, in_=in_), reads, writes)

    def memset(self, ap, val, writes, eng="pool"):
        self.S.op(eng, lambda e: e.memset(ap, val), (), writes)

    def dma(self, q, out, in_, reads, writes, dsem=None):
        self.S.dma(q, [(out, in_)], reads, writes, dsem)


def build_program(dbg=None):
    dbg = dbg or {}
    nc = bass.Bass("TRN2", target_bir_lowering=False)
    S = Sched(nc)
    k = K(nc, S)

    def din(name, shape, dt=F32):
        return nc.dram_tensor(name, list(shape), dt, kind="ExternalInput").ap()

    xb = din("xb", [SEQ, D])
    xs = din("xs", [NTOK_C, D])
    cT = din("cT", [128, 8])
    w_ada = din("w_ada", [D, 6 * D])
    b_ada = din("b_ada", [1, 6 * D])
    gain1 = din("gain1", [1, D])
    gain2 = din("gain2", [1, D])
    wA = din("wA", [D, 898])
    wG = din("wG", [D, 2048])
    qkg = din("qkg", [64, 2])
    lamv = din("lamv", [64, 4])
    onAc = din("onAc", [128, 1])
    onB = din("onB", [1, 128])
    convw = din("convw", [128, 12])
    adt = din("adt", [1, 2])
    w_ba = din("w_ba", [512, D])
    w_bb = din("w_bb", [512, D])
    w_out = din("w_out", [D, D])
    w_rt = din("w_rt", [D, 36])
    b_rt = din("b_rt", [1, 36])
    w1 = din("w1", [NEXP, D, DEXP])
    w3 = din("w3", [NEXP, D, DEXP])
    w2 = din("w2", [NEXP, DEXP, D])
    c_ident = din("c_ident", [128, 128])
    c_aug_q = din("c_aug_q", [3, SEQ])
    c_aug_k = din("c_aug_k", [3, SEQ])
    c_bias = din("c_bias", [128, 64])
    c_dmask = din("c_dmask", [128, 4 * 512])
    c_gmask = din("c_gmask", [128, 7 * 128])
    out = nc.dram_tensor("out", [NTOK_C, D], F32, kind="ExternalOutput").ap()

    exA_in = nc.dram_tensor("exA_in", [4, 128, NTOK_C], BF16)
    exB_in = nc.dram_tensor("exB_in", [4, 128, NTOK_C], BF16)
    if dbg.get("ex_out_input"):
        exA_out = nc.dram_tensor("exA_out", [4, 512, NTOK_C], BF16, kind="ExternalInput")
        exB_out = nc.dram_tensor("exB_out", [4, 512, NTOK_C], BF16, kind="ExternalInput")
    else:
        exA_out = nc.dram_tensor("exA_out", [4, 512, NTOK_C], BF16)
        exB_out = nc.dram_tensor("exB_out", [4, 512, NTOK_C], BF16)
    x1d = nc.dram_tensor("x1d", [NTOK_C, D], F32).ap()
    gdn_d = nc.dram_tensor("gdn_d", [3, 128, SEQ], BF16).ap()
    z_d = nc.dram_tensor("z_d", [SEQ, 128], F32).ap()
    dbg_outs = {}

    def dbg_out(name, shape, dt=F32):
        dbg_outs[name] = nc.dram_tensor(name, list(shape), dt, kind="ExternalOutput").ap()
        return dbg_outs[name]

    B_exA_in = [Buf("exA_in%d" % i) for i in range(4)]
    B_exB_in = [Buf("exB_in%d" % i) for i in range(4)]
    B_exA_out = Buf("exA_out")
    B_exB_out = Buf("exB_out")
    B_x1d = Buf("x1d")
    B_out = Buf("outd")

    with ExitStack() as st:
        def sb(stack, name, shape, dt=F32):
            return stack.enter_context(nc.sbuf_tensor(name, list(shape), dt))

        MOD = sb(st, "MOD", [128, 6 * D])
        B_MOD = Buf("MOD")
        identf = sb(st, "identf", [128, 128])
        identb = sb(st, "identb", [128, 128], BF16)
        onesf = sb(st, "onesf", [128, 128])
        epsc = sb(st, "epsc", [128, 1])
        neglam = sb(st, "neglam", [128, 1])
        B_const = Buf("const")
        B_neglam = Buf("neglam")
        BA = sb(st, "BA", [128, 64, 2])
        B_BA = Buf("BA")
        psb = [st.enter_context(nc.psum_tensor("ps%d" % i, [128, 512], F32)) for i in range(8)]
        B_ps = [Buf("ps%d" % i, excl=True) for i in range(8)]

        SH1 = MOD[:, 0:1024]
        G1 = MOD[:, 1024:2048]
        GATE1 = MOD[:, 2048:3072]
        SH2 = MOD[:, 3072:4096]
        G2 = MOD[:, 4096:5120]
        GATE2 = MOD[:, 5120:6144]

        with ExitStack() as s0:
            k.dma("sp", identf[:], c_ident, (), [B_const], dsem="c0")
            k.memset(onesf[:], 1.0, [B_const])
            k.memset(epsc[:], EPS, [B_const])
            k.copy(identb[:], identf[:], [B_const], [B_const], eng="dve")
            cv = sb(s0, "cv", [128, 8])
            sc = sb(s0, "sc", [128, 8])
            scb = sb(s0, "scb", [128, 8, 128])
            wst = [sb(s0, "wst%d" % i, [128, 8, 512]) for i in range(4)]
            g1t = sb(s0, "g1t", [128, D])
            g2t = sb(s0, "g2t", [128, D])
            lv = sb(s0, "lv", [64, 4])
            lp = sb(s0, "lp", [64, 2])
            le = sb(s0, "le", [128, 2])
            B_cv, B_sc, B_scb, B_g1t, B_g2t, B_lv, B_lp, B_le = [Buf(n) for n in
                                                              "cv sc scb g1t g2t lv lp le".split()]
            B_wst = [Buf("wst%d" % i) for i in range(4)]
            k.dma("sp", cv[:], cT, (), [B_cv])
            k.dma("sp", MOD[:], b_ada.partition_broadcast(128), (), [B_MOD])
            k.dma("sp", g1t[:], gain1.partition_broadcast(128), (), [B_g1t])
            k.dma("sp", g2t[:], gain2.partition_broadcast(128), (), [B_g2t])
            k.dma("sp", lv[:], lamv, (), [B_lv])
            k.act(sc[:], cv[:], AF.Silu, [B_cv], [B_sc])
            for kc in range(8):
                k.ts(scb[:, kc, :], onesf[:], sc[:, kc:kc + 1], ALU.mult, [B_sc, B_const], [B_scb])
            w_ada_v = w_ada.rearrange("(kc p) n -> p kc n", p=128)
            def ada_load(nb):
                k.dma("sp" if nb % 2 == 0 else "act", wst[nb % 4][:], w_ada_v[:, :, nb * 512:(nb + 1) * 512], (), [B_wst[nb % 4]])
            for nb in range(3):
                ada_load(nb)
            for nb in range(12):
                wb = nb % 4
                if nb + 3 < 12:
                    ada_load(nb + 3)
                pb = nb % 2
                for kc in range(8):
                    k.mm(psb[pb][:, :], scb[:, kc, :], wst[wb][:, kc, :], kc == 0, kc == 7,
                         [B_scb, B_wst[wb]], [B_ps[pb]])
                k.tt(MOD[:, nb * 512:(nb + 1) * 512], psb[pb][:, :], MOD[:, nb * 512:(nb + 1) * 512], ALU.add,
                     [B_ps[pb], B_MOD], [B_MOD])
            k.stt(G1, G1, 1.0, g1t[:], ALU.add, ALU.mult, [B_MOD, B_g1t], [B_MOD])
            k.stt(G2, G2, 1.0, g2t[:], ALU.add, ALU.mult, [B_MOD, B_g2t], [B_MOD])
            k.tt(lp[:, 0:1], lv[:, 0:1], lv[:, 1:2], ALU.mult, [B_lv], [B_lp])
            k.tt(lp[:, 1:2], lv[:, 2:3], lv[:, 3:4], ALU.mult, [B_lv], [B_lp])
            k.mm(psb[2][:, 0:2], onesf[0:64, :], lp[:, :], True, True, [B_lp, B_const], [B_ps[2]])
            k.act(le[:], psb[2][:, 0:2], AF.Exp, [B_ps[2]], [B_le])
            k.tt(neglam[:], le[:, 1:2], le[:, 0:1], ALU.subtract, [B_le], [B_neglam])
            k.ts(neglam[:], neglam[:], -LAMBDA_INIT, ALU.add, [B_neglam], [B_neglam])
            if dbg.get("out_mod"):
                o = dbg_out("d_mod", [128, 6 * D])
                k.dma("sp", o, MOD[:], [B_MOD], [Buf("d_mod")])
                o2 = dbg_out("d_lam", [128, 1])
                k.dma("sp", o2, neglam[:], [B_neglam], [Buf("d_lam")])
            S.flush(st)

        ctx = dict(nc=nc, S=S, k=k, st=st, sb=sb, MOD=MOD, B_MOD=B_MOD, identf=identf, identb=identb, onesf=onesf,
                   epsc=epsc, neglam=neglam, B_const=B_const, B_neglam=B_neglam, psb=psb, B_ps=B_ps,
                   BA=BA, B_BA=B_BA, B_gdn=Buf("gdn_d"), B_zd=Buf("z_d"),
                   SH1=SH1, G1=G1, GATE1=GATE1, SH2=SH2, G2=G2, GATE2=GATE2, dbg=dbg, dbg_out=dbg_out)
        io = dict(xb=xb, xs=xs, wA=wA, wG=wG, qkg=qkg, onAc=onAc, onB=onB, convw=convw, adt=adt, w_ba=w_ba, w_bb=w_bb,
                  w_out=w_out, w_rt=w_rt, b_rt=b_rt, w1=w1, w3=w3, w2=w2, c_aug_q=c_aug_q, c_aug_k=c_aug_k,
                  c_bias=c_bias, c_dmask=c_dmask, c_gmask=c_gmask, out=out, exA_in=exA_in, exB_in=exB_in, exA_out=exA_out,
                  exB_out=exB_out, x1d=x1d, gdn_d=gdn_d, z_d=z_d, B_exA_in=B_exA_in, B_exB_in=B_exB_in,
                  B_exA_out=B_exA_out, B_exB_out=B_exB_out, B_x1d=B_x1d, B_out=B_out)

        def gather1(ein, eout, B_in, B_o, tag, sl):
            def ccfn(e):
                return e.collective_compute("AllGather", ALU.bypass, replica_groups=[[0, 1, 2, 3], [4, 5, 6, 7]],
                                            ins=[ein.ap()[sl].opt()], outs=[eout.ap()[sl].opt()])
            S.cc(ccfn, [B_in[sl]], [B_o], dsem="cc_%s%d" % (tag, sl))

        if not dbg.get("ex_out_input"):
            ctx["gatherA"] = lambda sl: gather1(exA_in, exA_out, B_exA_in, B_exA_out, "a", sl)
            ctx["gatherB"] = lambda sl: gather1(exB_in, exB_out, B_exB_in, B_exB_out, "b", sl)
        if not dbg.get("skip_p1"):
            phase1(ctx, io)
        if not dbg.get("skip_p2"):
            phase2(ctx, io)
        if dbg.get("out_gdn"):
            o = dbg_out("d_gdn", [3, 128, SEQ], BF16)
            k.dma("sp", o, gdn_d, [ctx["B_gdn"]], [Buf("d_gdn")])
            o = dbg_out("d_z", [SEQ, 128])
            k.dma("sp", o, z_d, [ctx["B_zd"]], [Buf("d_z")])
            o = dbg_out("d_ba", [128, 128])
            k.dma("sp", o, BA[:].rearrange("p a b -> p (a b)"), [B_BA], [Buf("d_ba")])
        if dbg.get("out_exin"):
            o = dbg_out("d_exin", [2, 4, 128, NTOK_C], BF16)
            k.dma("sp", o[0], exA_in.ap(), B_exA_in, [Buf("d_exinA")])
            k.dma("sp", o[1], exB_in.ap(), B_exB_in, [Buf("d_exinB")])
        if not dbg.get("skip_p3"):
            phase3(ctx, io)
        S.flush(st, final=True)
    return nc, dbg_outs


def norm_mod(ctx, xt, B_xt, G, SH, junk, B_junk, ssq, B_ssq, tmp, B_tmp, out_ap, B_out, out_eng="dve"):
    k = ctx["k"]
    k.act(junk, xt, AF.Square, [B_xt], [B_junk, B_ssq], accum_out=ssq[:, 0:1])
    k.act(ssq[:, 1:2], ssq[:, 0:1], AF.Ln, [B_ssq], [B_ssq], scale=1.0 / D, bias=ctx["epsc"][:, 0:1])
    k.act(ssq[:, 2:3], ssq[:, 1:2], AF.Exp, [B_ssq], [B_ssq], scale=-0.5)
    k.stt(tmp, xt, ssq[:, 2:3], G, ALU.mult, ALU.mult, [B_xt, B_ssq, ctx["B_MOD"]], [B_tmp])
    k.tt(out_ap, tmp, SH, ALU.add, [B_tmp, ctx["B_MOD"]], [B_out], eng=out_eng)


def transpose_block_bf(ctx, hb, B_hb, dst, B_dst, pbank, evac_eng):
    k = ctx["k"]
    ps = ctx["psb"][pbank]
    B_p = ctx["B_ps"][pbank]
    pv = ps[:, :].bitcast(BF16)
    for kc in range(8):
        k.tr(pv[:, kc * 128:(kc + 1) * 128], hb[:, kc * 128:(kc + 1) * 128], ctx["identb"][:], [B_hb, ctx["B_const"]], [B_p])
    src = pv.rearrange("p (kc t) -> p kc t", kc=8)
    if evac_eng == "act":
        k.copy(dst, src, [B_p], [B_dst], eng="act")
    else:
        k.copy(dst, src, [B_p], [B_dst], eng=evac_eng)


def phase3(ctx, io):
    nc, S, k, st, sb = ctx["nc"], ctx["S"], ctx["k"], ctx["st"], ctx["sb"]
    psb, B_ps = ctx["psb"], ctx["B_ps"]
    dbg = ctx["dbg"]
    exA2 = io["exA_out"].ap().rearrange("s r n -> (s r) n")
    exB2 = io["exB_out"].ap().rearrange("s r n -> (s r) n")
    with ExitStack() as s3:
        H2T = sb(s3, "H2T", [128, 8, NTOK_C], BF16)
        B_H2T = [Buf("H2T%d" % i) for i in range(16)]
        GW = sb(s3, "GW", [128, 16, 32])
        B_GW = [Buf("GW%d" % i) for i in range(16)]
        with ExitStack() as s31:
            WGt = [sb(s31, "WGt%d" % i, [128, 8, 512], BF16) for i in range(2)]
            B_WGt = [Buf("WGt0"), Buf("WGt1")]
            WAt = sb(s31, "WAt", [128, 4, D], BF16)
            WBt = sb(s31, "WBt", [128, 4, D], BF16)
            WOt = sb(s31, "WOt", [128, 8, D], BF16)
            WRt = sb(s31, "WRt", [128, 8, 36])
            brt = sb(s31, "brt", [128, 36])
            B_W = Buf("p3w")
            XT = sb(s31, "XT", [128, 4, D])
            B_XT = [Buf("XT%d" % i) for i in range(4)]
            junk = sb(s31, "junk3", [128, D], BF16)
            B_junk = Buf("junk3")
            ssq = sb(s31, "ssq3", [128, 4])
            B_ssq = Buf("ssq3")
            hT = sb(s31, "hT3", [128, 8, 512], BF16)
            B_hT = Buf("hT3")
            GT = sb(s31, "GT", [128, 16, 512], BF16)
            B_GT = Buf("GT")
            OAB = sb(s31, "OAB", [128, 4, 2, 512], BF16)
            B_OAB = Buf("OAB")
            MT = sb(s31, "MT", [128, 8, 512], BF16)
            B_MT = Buf("MT")
            t1 = t2 = B_t1 = B_t2 = None
            x1t = [sb(s31, "x1t%d" % i, [128, D]) for i in range(2)]
            B_x1t = [Buf("x1t0"), Buf("x1t1")]
            B_rl = Buf("rl")
            B_rw = Buf("rw")
            def mk2(name, shape, dt=F32):
                return [sb(s31, "%s_%d" % (name, i), shape, dt) for i in range(2)], [Buf("%s_%d" % (name, i)) for i in range(2)]
            tmpf2, B_tmpf2 = mk2("tmpf2", [128, D])
            junk2, B_junk2 = [junk, junk], [B_junk, Buf("junk3b")]
            ssq2, B_ssq2 = mk2("ssq2", [128, 4])
            ssq4 = sb(s31, "ssq4_3", [128, 4, 4])
            B_ssq4 = [Buf("ssq4_3_%d" % i) for i in range(4)]
            h2f2, B_h2f2 = mk2("h2f2", [128, D])
            h2b2, B_h2b2 = mk2("h2b2", [128, D], BF16)
            h2T322, B_h2T322 = mk2("h2T322", [128, 8, 128])
            t1 = [tmpf2[1][:, 0:512], tmpf2[0][:, 0:512]]
            t2 = [tmpf2[1][:, 512:1024], tmpf2[0][:, 512:1024]]
            B_t1 = [B_tmpf2[1], B_tmpf2[0]]
            B_t2 = [B_tmpf2[1], B_tmpf2[0]]
            hb, B_hb = h2b2, B_h2b2
            tmpf, B_tmpf = tmpf2[0], B_tmpf2[0]
            rl2, B_rl2 = mk2("rl2", [128, 36])
            rw2, B_rw2 = mk2("rw2", [128, 160])

            k.dma("pool", WAt[:], io["w_ba"].rearrange("(h p) n -> p h n", p=128), (), [B_W], dsem="p3w_a")
            k.dma("pool", WBt[:], io["w_bb"].rearrange("(h p) n -> p h n", p=128), (), [B_W], dsem="p3w_b")
            k.dma("pool", WOt[:], io["w_out"].rearrange("(kc p) n -> p kc n", p=128), (), [B_W], dsem="p3w_o")
            k.dma("sp", WRt[:], io["w_rt"].rearrange("(kc p) n -> p kc n", p=128), (), [B_W], dsem="p3w_r")
            k.dma("sp", brt[:], io["b_rt"].partition_broadcast(128), (), [B_W], dsem="p3w_rb")
            wG_v = io["wG"].rearrange("(kc p) n -> p kc n", p=128)
            wgi = 0
            jcache = {}
            for t in range(4):
                for blk in range(4):
                    gb = 4 * t + blk
                    k.dma("sp", XT[:, blk, :], io["xs"][gb * 128:(gb + 1) * 128, :], (), [B_XT[blk]])
                for blk in range(4):
                    k.act(junk[:], XT[:, blk, :], AF.Square, [B_XT[blk]], [B_junk, B_ssq4[blk]], accum_out=ssq4[:, blk, 0:1])
                for blk in range(4):
                    k.act(ssq4[:, blk, 1:2], ssq4[:, blk, 0:1], AF.Ln, [B_ssq4[blk]], [B_ssq4[blk]], scale=1.0 / D, bias=ctx["epsc"][:, 0:1])
                for blk in range(4):
                    k.act(ssq4[:, blk, 2:3], ssq4[:, blk, 1:2], AF.Exp, [B_ssq4[blk]], [B_ssq4[blk]], scale=-0.5)
                for blk in range(4):
                    hbi = blk % 2
                    k.stt(tmpf[:], XT[:, blk, :], ssq4[:, blk, 2:3], ctx["G1"], ALU.mult, ALU.mult, [B_XT[blk], B_ssq4[blk], ctx["B_MOD"]], [B_tmpf])
                    k.tt(hb[hbi][:], tmpf[:], ctx["SH1"], ALU.add, [B_tmpf, ctx["B_MOD"]], [B_hb[hbi]])
                    transpose_block_bf(ctx, hb[hbi], B_hb[hbi], hT[:, :, blk * 128:(blk + 1) * 128], B_hT,
                                       pbank=blk % 2, evac_eng="act" if blk % 2 == 0 else "dve")
                for gq in range(4):
                    wb = wgi % 2
                    wgi += 1
                    k.dma("pool", WGt[wb][:], wG_v[:, :, gq * 512:(gq + 1) * 512], (), [B_WGt[wb]])
                    for gc in range(4):
                        pb = 2 + (gc % 2)
                        for kc in range(8):
                            k.mm(psb[pb][:, :], WGt[wb][:, kc, gc * 128:(gc + 1) * 128], hT[:, kc, :], kc == 0, kc == 7,
                                 [B_WGt[wb], B_hT], [B_ps[pb]])
                        k.act(GT[:, gq * 4 + gc, :], psb[pb][:, :], AF.Sigmoid, [B_ps[pb]], [B_GT])
                pairs = []
                for r_ in range(4):
                    for two, ex2 in enumerate((exA2, exB2)):
                        def src_fn(eng, t=t, r_=r_, ex2=ex2):
                            if "j" not in jcache:
                                jcache["j"] = eng.partition_id() % 4
                            return ex2[bass.ds(jcache["j"] * 512 + r_ * 128, 128), t * 512:(t + 1) * 512]
                        pairs.append((OAB[:, r_, two, :], src_fn))
                S.dma("pool", pairs, [io["B_exA_out"], io["B_exB_out"]], [B_OAB])
                for dc in range(8):
                    i2 = dc % 2
                    pa_, pb_ = (4, 5) if i2 == 0 else (6, 7)
                    for r in range(4):
                        k.mm(psb[pa_][:, :], WAt[:, r, dc * 128:(dc + 1) * 128], OAB[:, r, 0, :], r == 0, r == 3,
                             [B_W, B_OAB], [B_ps[pa_]])
                    for r in range(4):
                        k.mm(psb[pb_][:, :], WBt[:, r, dc * 128:(dc + 1) * 128], OAB[:, r, 1, :], r == 0, r == 3,
                             [B_W, B_OAB], [B_ps[pb_]])
                    k.tt(t1[i2], GT[:, dc, :], psb[pa_][:, :], ALU.mult, [B_GT, B_ps[pa_]], [B_t1[i2]])
                    k.tt(t2[i2], GT[:, 8 + dc, :], psb[pb_][:, :], ALU.mult, [B_GT, B_ps[pb_]], [B_t2[i2]])
                    k.tt(MT[:, dc, :], t1[i2], t2[i2], ALU.add, [B_t1[i2], B_t2[i2]], [B_MT])
                def blockchain(blk, sl):
                    gb = 4 * t + blk
                    pbo = 4 + 2 * sl
                    for nb in range(2):
                        pb = pbo + nb
                        for dc in range(8):
                            k.mm(psb[pb][:, :], MT[:, dc, blk * 128:(blk + 1) * 128], WOt[:, dc, nb * 512:(nb + 1) * 512],
                                 dc == 0, dc == 7, [B_MT, B_W], [B_ps[pb]])
                        yield
                        k.tt(tmpf2[sl][:, nb * 512:(nb + 1) * 512], psb[pb][:, :], ctx["GATE1"][:, nb * 512:(nb + 1) * 512], ALU.mult,
                             [B_ps[pb], ctx["B_MOD"]], [B_tmpf2[sl]])
                        yield
                    k.tt(x1t[sl][:], tmpf2[sl][:], XT[:, blk, :], ALU.add, [B_tmpf2[sl], B_XT[blk]], [B_x1t[sl]])
                    k.dma("sp", io["x1d"][gb * 128:(gb + 1) * 128, :], x1t[sl][:], [B_x1t[sl]], [io["B_x1d"]], dsem="x1d_w%d" % sl)
                    yield
                    k.act(junk2[sl][:], x1t[sl][:], AF.Square, [B_x1t[sl]], [B_junk2[sl], B_ssq2[sl]], accum_out=ssq2[sl][:, 0:1])
                    yield
                    k.act(ssq2[sl][:, 1:2], ssq2[sl][:, 0:1], AF.Ln, [B_ssq2[sl]], [B_ssq2[sl]], scale=1.0 / D, bias=ctx["epsc"][:, 0:1])
                    k.act(ssq2[sl][:, 2:3], ssq2[sl][:, 1:2], AF.Exp, [B_ssq2[sl]], [B_ssq2[sl]], scale=-0.5)
                    yield
                    k.stt(tmpf2[sl][:], x1t[sl][:], ssq2[sl][:, 2:3], ctx["G2"], ALU.mult, ALU.mult, [B_x1t[sl], B_ssq2[sl], ctx["B_MOD"]],
                          [B_tmpf2[sl]])
                    yield
                    k.tt(h2f2[sl][:], tmpf2[sl][:], ctx["SH2"], ALU.add, [B_tmpf2[sl], ctx["B_MOD"]], [B_h2f2[sl]])
                    yield
                    k.copy(h2b2[sl][:], h2f2[sl][:], [B_h2f2[sl]], [B_h2b2[sl]], eng="act")
                    yield
                    pv = psb[sl][:, :].bitcast(BF16)
                    for kc in range(8):
                        k.tr(pv[:, kc * 128:(kc + 1) * 128], h2b2[sl][:, kc * 128:(kc + 1) * 128], ctx["identb"][:],
                             [B_h2b2[sl], ctx["B_const"]], [B_ps[sl]])
                    yield
                    k.copy(H2T[:, :, gb * 128:(gb + 1) * 128], pv.rearrange("p (kc t) -> p kc t", kc=8), [B_ps[sl]], [B_H2T[gb]], eng="act")
                    yield
                    for half in range(2):
                        pb = 2 + sl
                        for q4 in range(4):
                            kc = half * 4 + q4
                            k.tr(psb[pb][:, q4 * 128:(q4 + 1) * 128], h2f2[sl][:, kc * 128:(kc + 1) * 128], ctx["identf"][:],
                                 [B_h2f2[sl], ctx["B_const"]], [B_ps[pb]])
                        yield
                        k.copy(h2T322[sl][:, half * 4:(half + 1) * 4, :], psb[pb][:, :].rearrange("p (a b) -> p a b", a=4),
                               [B_ps[pb]], [B_h2T322[sl]], eng="dve" if half == 0 else "act")
                        yield
                    for kc in range(8):
                        k.mm(psb[pbo][:, 0:36], h2T322[sl][:, kc, :], WRt[:, kc, :], kc == 0, kc == 7, [B_h2T322[sl], B_W], [B_ps[pbo]])
                    yield
                    k.tt(rl2[sl][:], psb[pbo][:, 0:36], brt[:], ALU.add, [B_ps[pbo], B_W], [B_rl2[sl]])
                    yield
                    yield from routing(ctx, rl2[sl], B_rl2[sl], rw2[sl], B_rw2[sl], GW[:, gb, :], B_GW[gb])

                for pair in range(2):
                    gens = [blockchain(2 * pair, 0), blockchain(2 * pair + 1, 1)]
                    alive = [True, True]
                    while any(alive):
                        for gi_ in range(2):
                            if alive[gi_]:
                                try:
                                    next(gens[gi_])
                                except StopIteration:
                                    alive[gi_] = False
            S.flush(st)
        if dbg.get("out_gw"):
            o = ctx["dbg_out"]("d_gw", [128, 16 * 32])
            k.dma("sp", o, GW[:].rearrange("p a b -> p (a b)"), B_GW, [Buf("d_gw")])
        with ExitStack() as s32:
            ACC = sb(s32, "ACC", [128, 16, D])
            B_ACC = [Buf("ACC%d" % i) for i in range(16)]
            EW1 = [sb(s32, "EW1_%d" % i, [128, 8, DEXP], BF16) for i in range(2)]
            EW3 = [sb(s32, "EW3_%d" % i, [128, 8, DEXP], BF16) for i in range(2)]
            EW2 = [sb(s32, "EW2_%d" % i, [128, 4, D], BF16) for i in range(2)]
            B_EW1 = [Buf("EW1_0"), Buf("EW1_1")]
            B_EW3 = [Buf("EW3_0"), Buf("EW3_1")]
            B_EW2 = [Buf("EW2_0"), Buf("EW2_1")]
            HID = [sb(s32, "HID%d" % i, [128, 4, 512], BF16) for i in range(2)]
            B_HID = [Buf("HID0"), Buf("HID1")]
            SIL = [sb(s32, "SIL%d" % i, [128, 512]) for i in range(2)]
            B_SIL = [Buf("SIL0"), Buf("SIL1")]
            xo = [sb(s32, "xo%d" % i, [128, D]) for i in range(2)]
            B_xo = [Buf("xo0"), Buf("xo1")]
            for e in range(NEXP):
                eb = e % 2
                k.dma("pool", EW1[eb][:], io["w1"][e].rearrange("(kc p) n -> p kc n", p=128), (), [B_EW1[eb]])
                k.dma("pool", EW3[eb][:], io["w3"][e].rearrange("(kc p) n -> p kc n", p=128), (), [B_EW3[eb]])
                k.dma("pool", EW2[eb][:], io["w2"][e].rearrange("(fc p) n -> p fc n", p=128), (), [B_EW2[eb]])
                for tt_ in range(4):
                    hi = (e * 4 + tt_) % 2
                    for fc in range(4):
                        pa = (fc % 2) * 2
                        for kc in range(8):
                            k.mm(psb[pa][:, :], EW1[eb][:, kc, fc * 128:(fc + 1) * 128], H2T[:, kc, tt_ * 512:(tt_ + 1) * 512],
                                 kc == 0, kc == 7, [B_EW1[eb]] + B_H2T[tt_ * 4:(tt_ + 1) * 4], [B_ps[pa]])
                        for kc in range(8):
                            k.mm(psb[pa + 1][:, :], EW3[eb][:, kc, fc * 128:(fc + 1) * 128], H2T[:, kc, tt_ * 512:(tt_ + 1) * 512],
                                 kc == 0, kc == 7, [B_EW3[eb]] + B_H2T[tt_ * 4:(tt_ + 1) * 4], [B_ps[pa + 1]])
                        si = fc % 2
                        k.act(SIL[si][:], psb[pa][:, :], AF.Silu, [B_ps[pa]], [B_SIL[si]])
                        k.tt(HID[hi][:, fc, :], SIL[si][:], psb[pa + 1][:, :], ALU.mult, [B_SIL[si], B_ps[pa + 1]], [B_HID[hi]])
                    for blk in range(4):
                        gb = tt_ * 4 + blk
                        for nb in range(2):
                            pb = 4 + (2 * blk + nb) % 4
                            for fc in range(4):
                                k.mm(psb[pb][:, :], HID[hi][:, fc, blk * 128:(blk + 1) * 128], EW2[eb][:, fc, nb * 512:(nb + 1) * 512],
                                     fc == 0, fc == 3, [B_HID[hi], B_EW2[eb]], [B_ps[pb]])
                            dst = ACC[:, gb, nb * 512:(nb + 1) * 512]
                            if e == 0:
                                k.ts(dst, psb[pb][:, :], GW[:, gb, e:e + 1], ALU.mult, [B_ps[pb], B_GW[gb]], [B_ACC[gb]])
                            else:
                                k.stt(dst, psb[pb][:, :], GW[:, gb, e:e + 1], dst, ALU.mult, ALU.add,
                                      [B_ps[pb], B_GW[gb], B_ACC[gb]], [B_ACC[gb]])
            for gb in range(16):
                xi = gb % 2
                k.dma("sp", xo[xi][:], io["x1d"][gb * 128:(gb + 1) * 128, :], [io["B_x1d"]], [B_xo[xi]])
                k.tt(ACC[:, gb, :], ACC[:, gb, :], ctx["GATE2"], ALU.mult, [B_ACC[gb], ctx["B_MOD"]], [B_ACC[gb]])
                k.tt(xo[xi][:], xo[xi][:], ACC[:, gb, :], ALU.add, [B_xo[xi], B_ACC[gb]], [B_xo[xi]])
                k.dma("sp", io["out"][gb * 128:(gb + 1) * 128, :], xo[xi][:], [B_xo[xi]], [io["B_out"]], dsem="out_w%d" % xi)
            S.flush(st)


def routing(ctx, rl, B_rl, rw, B_rw, gw_out, B_gw):
    k = ctx["k"]
    R, W = [B_rl, B_rw], [B_rw]
    gmax = rw[:, 0:1]
    ge = rw[:, 1:5]
    gsum = rw[:, 5:6]
    gtp = rw[:, 6:7]
    ohg = rw[:, 8:12]
    pen = rw[:, 12:16]
    msk = rw[:, 16:48]
    m1 = rw[:, 48:49]
    oh1 = rw[:, 49:81]
    msk2 = rw[:, 81:113]
    m2 = rw[:, 113:114]
    oh2 = rw[:, 114:146]
    dd = rw[:, 146:147]
    p1 = rw[:, 147:148]
    p2 = rw[:, 148:149]
    ngmax = rw[:, 149:150]
    S = ctx["S"]
    S.op("dve", lambda e: e.reduce_max(out=gmax, in_=rl[:, 0:4], axis=AX.X), [B_rl], W)
    yield
    k.ts(ngmax, gmax, -1.0, ALU.mult, R, W)
    yield
    k.act(ge, rl[:, 0:4], AF.Exp, R, W, bias=ngmax, accum_out=gsum)
    yield
    k.recip(gtp, gsum, R, W)
    yield
    k.ts(ohg, rl[:, 0:4], gmax, ALU.is_equal, R, W)
    yield
    k.ts(pen, ohg, -1.0, ALU.add, R, W, s2=-NEG, op1=ALU.mult)
    yield
    k.tt(msk.rearrange("p (g e) -> p g e", g=4), rl[:, 4:36].rearrange("p (g e) -> p g e", g=4),
         pen.unsqueeze(2).to_broadcast([128, 4, 8]), ALU.add, R, W)
    yield
    S.op("dve", lambda e: e.reduce_max(out=m1, in_=msk, axis=AX.X), R, W)
    yield
    k.ts(oh1, msk, m1, ALU.is_equal, R, W)
    yield
    k.stt(msk2, oh1, NEG, msk, ALU.mult, ALU.add, R, W)
    yield
    S.op("dve", lambda e: e.reduce_max(out=m2, in_=msk2, axis=AX.X), R, W)
    yield
    k.ts(oh2, msk2, m2, ALU.is_equal, R, W)
    yield
    k.tt(dd, m2, m1, ALU.subtract, R, W)
    yield
    k.act(dd, dd, AF.Exp, R, W)
    yield
    k.ts(p1, dd, 1.0, ALU.add, R, W)
    yield
    k.recip(p1, p1, R, W)
    yield
    k.tt(p2, dd, p1, ALU.mult, R, W)
    yield
    k.tt(p1, p1, gtp, ALU.mult, R, W)
    yield
    k.tt(p2, p2, gtp, ALU.mult, R, W)
    yield
    k.ts(oh1, oh1, p1, ALU.mult, R, W)
    yield
    k.stt(gw_out, oh2, p2, oh1, ALU.mult, ALU.add, R, [B_gw])
    yield


def phase1(ctx, io):
    nc, S, k, st, sb = ctx["nc"], ctx["S"], ctx["k"], ctx["st"], ctx["sb"]
    psb, B_ps = ctx["psb"], ctx["B_ps"]
    dbg = ctx["dbg"]
    BA, B_BA = ctx["BA"], ctx["B_BA"]
    onesf, epsc, B_const = ctx["onesf"], ctx["epsc"], ctx["B_const"]
    NT = dbg.get("p1_tiles", 16)
    with ExitStack() as s1:
        KT = [sb(s1, "KT%d" % m, [67, SEQ], BF16) for m in range(2)]
        B_KT = [[Buf("KT%d_%d" % (m, t)) for t in range(16)] for m in range(2)]
        B_KTaug = Buf("KTaug")
        QT = [sb(s1, "QT%d" % m, [67, SEQ], BF16) for m in range(2)]
        B_QT = [[Buf("QT%d_%d" % (m, t)) for t in range(16)] for m in range(2)]
        B_QTaug = Buf("QTaug")
        VA = sb(s1, "VA", [128, 64, 128], BF16)
        B_VA = [Buf("VA%d" % t) for t in range(16)]
        qkgt = sb(s1, "qkgt", [64, 3])
        onAc = sb(s1, "onAc_sb", [128, 1])
        onesb = sb(s1, "onesb", [128, 128], BF16)
        cwt = sb(s1, "cwt", [128, 12])
        B_c1 = Buf("p1const")
        k.dma("sp", qkgt[:, 0:2], io["qkg"], (), [B_c1], dsem="p1c2")
        k.dma("sp", onAc[:], io["onAc"], (), [B_c1], dsem="p1c3")
        k.dma("sp", cwt[:], io["convw"], (), [B_c1], dsem="p1c4")
        k.ts(qkgt[:, 2:3], qkgt[:, 0:1], 0.125, ALU.mult, [B_c1], [B_c1])
        k.copy(onesb[:], onesf[:], [B_const], [B_c1], eng="dve")
        k.ts(onAc[:], onAc[:], 1.0 - LAMBDA_INIT, ALU.mult, [B_c1], [B_c1])
        for m in range(2):
            k.dma("pool", KT[m][64:67, :], io["c_aug_k"], (), [B_KTaug], dsem="p1aug_k%d" % m)
            k.dma("pool", QT[m][64:67, :], io["c_aug_q"], (), [B_QTaug], dsem="p1aug_q%d" % m)

        with ExitStack() as s1a:
            WAb = sb(s1a, "WAb", [128, 8, 898], BF16)
            B_WAb = Buf("WAb")
            k.dma("pool", WAb[:], io["wA"].rearrange("(kc p) n -> p kc n", p=128), (), [B_WAb])
            xt = [sb(s1a, "xt%d" % i, [128, D]) for i in range(4)]
            B_xt = [Buf("xt%d" % i) for i in range(4)]
            ssq4 = sb(s1a, "ssq4", [128, 4, 4])
            B_ssq4 = [Buf("ssq4_%d" % i) for i in range(4)]
            junk = sb(s1a, "junk1", [128, D], BF16)
            B_junk = Buf("junk1")
            tmpf = sb(s1a, "tmpf1", [128, D])
            B_tmpf = Buf("tmpf1")
            ssq = sb(s1a, "ssq1", [128, 4])
            B_ssq = Buf("ssq1")
            hb = [sb(s1a, "hb1_%d" % i, [128, D], BF16) for i in range(2)]
            B_hb = [Buf("hb1_0"), Buf("hb1_1")]
            hT = [sb(s1a, "hT1_%d" % i, [128, 8, 512], BF16) for i in range(2)]
            B_hT = [Buf("hT1_0"), Buf("hT1_1")]
            _sq = [sb(s1a, "sq%d" % i, [128, 512]) for i in range(4)]
            sq = [_sq[0], _sq[1], None, None, _sq[2], _sq[3]]
            B_sq = [Buf("sq%d" % i) for i in range(6)]
            CONVIN = sb(s1a, "CONVIN", [128, 3, 515])
            B_cin = [Buf("cin%d" % i) for i in range(3)]
            cacc = [sb(s1a, "cacc%d" % i, [128, 512]) for i in range(3)]
            csil = [sb(s1a, "csil%d" % i, [128, 512]) for i in range(3)]
            B_cacc = [Buf("cacc%d" % i) for i in range(3)]
            B_csil = [Buf("csil%d" % i) for i in range(3)]
            gst = [sb(s1a, "gst%d" % i, [128, 3, 512], BF16) for i in range(2)]
            B_gst = [Buf("gst0"), Buf("gst1")]
            zst = [sb(s1a, "zst%d" % i, [128, 4, 128]) for i in range(2)]
            B_zst = [Buf("zst0"), Buf("zst1")]
            k.memset(CONVIN[:, :, 0:3], 0.0, B_cin, eng="pool")

            fm_groups = [("q", 0, 0, 128), ("k", 0, 128, 128), ("g", 0, 256, 128), ("g", 1, 384, 128), ("g", 2, 512, 128)]
            fm_bank = [0, 1, 2, 3, 4]
            xv = io["xb"]
            sqb = [sb(s1a, "sqb%d" % i, [128, 512], BF16) for i in range(4)]
            B_sqb = [Buf("sqb%d" % i) for i in range(4)]
            QS = [sb(s1a, "QS%d" % i, [128, 512], BF16) for i in range(4)]
            B_QS = [Buf("QS%d" % i) for i in range(4)]
            gain2 = sb(s1a, "gain2c", [128, 2])
            bd = sb(s1a, "bd64", [128, 128], BF16)
            k.memset(bd[:], 0.0, [B_c1], eng="dve")
            k.memset(bd[0:64, 0:64], 1.0, [B_c1], eng="dve")
            k.memset(bd[64:128, 64:128], 1.0, [B_c1], eng="dve")
            k.dma("sp", gain2[0:64, :], io["qkg"], (), [B_c1], dsem="p1c5")
            k.dma("sp", gain2[64:128, :], io["qkg"], (), [B_c1], dsem="p1c6")
            k.ts(gain2[:, 0:1], gain2[:, 0:1], 0.125, ALU.mult, [B_c1], [B_c1])

            def part1(t):
                hi = t % 2
                for blk in range(4):
                    gb = 4 * t + blk
                    k.dma("sp", xt[blk][:], xv[gb * 128:(gb + 1) * 128, :], (), [B_xt[blk]])
                for blk in range(4):
                    k.act(junk[:], xt[blk][:], AF.Square, [B_xt[blk]], [B_junk, B_ssq4[blk]], accum_out=ssq4[:, blk, 0:1])
                for blk in range(4):
                    k.act(ssq4[:, blk, 1:2], ssq4[:, blk, 0:1], AF.Ln, [B_ssq4[blk]], [B_ssq4[blk]], scale=1.0 / D, bias=epsc[:, 0:1])
                for blk in range(4):
                    k.act(ssq4[:, blk, 2:3], ssq4[:, blk, 1:2], AF.Exp, [B_ssq4[blk]], [B_ssq4[blk]], scale=-0.5)
                for blk in range(4):
                    hbi = blk % 2
                    k.stt(tmpf[:], xt[blk][:], ssq4[:, blk, 2:3], ctx["G1"], ALU.mult, ALU.mult, [B_xt[blk], B_ssq4[blk], ctx["B_MOD"]], [B_tmpf])
                    k.tt(hb[hbi][:], tmpf[:], ctx["SH1"], ALU.add, [B_tmpf, ctx["B_MOD"]], [B_hb[hbi]])
                    pbk = 7 if blk % 2 == 0 else 6
                    pv = psb[pbk][:, :].bitcast(BF16)
                    for kc in range(8):
                        k.tr(pv[:, kc * 128:(kc + 1) * 128], hb[hbi][:, kc * 128:(kc + 1) * 128], ctx["identb"][:], [B_hb[hbi], B_const], [B_ps[pbk]])
                    k.copy(hT[hi][:, :, blk * 128:(blk + 1) * 128], pv.rearrange("p (kc t) -> p kc t", kc=8), [B_ps[pbk]], [B_hT[hi]],
                           eng="act" if blk % 2 == 0 else "dve")

            def part2(t):
                hi = t % 2
                for gi_, (kind, idx, c0, M) in enumerate(fm_groups):
                    for kc in range(8):
                        k.mm(psb[gi_][0:M, :], WAb[:, kc, c0:c0 + M], hT[hi][:, kc, :], kc == 0, kc == 7, [B_WAb, B_hT[hi]], [B_ps[gi_]])

            def part3(t):
                par = t % 2
                for gi_ in range(2):
                    si = 2 * par + gi_
                    k.act(sqb[si][:], psb[gi_][:, :], AF.Square, [B_ps[gi_]], [B_sqb[si]])
                for gi_ in range(2, 5):
                    ch = gi_ - 2
                    k.copy(CONVIN[:, ch, 3:515], psb[gi_][:, :], [B_ps[gi_]], [B_cin[ch]], eng="dve" if ch == 0 else "act")
                for ch in range(3):
                    k.act(cacc[ch][:], CONVIN[:, ch, 0:512], AF.Identity, [B_cin[ch], B_c1], [B_cacc[ch]],
                          scale=cwt[:, ch * 4:ch * 4 + 1])
                for gi_ in range(2):
                    si = 2 * par + gi_
                    k.mm(psb[5][:, :], bd[:, :], sqb[si][:], True, True, [B_sqb[si], B_c1], [B_ps[5]])
                    k.act(sq[gi_][:], psb[5][:, :], AF.Ln, [B_ps[5]], [B_sq[gi_]], scale=1.0 / 64, bias=epsc[:, 0:1])
                for j in range(1, 4):
                    for ch in range(3):
                        k.stt(cacc[ch][:], CONVIN[:, ch, j:j + 512], cwt[:, ch * 4 + j:ch * 4 + j + 1], cacc[ch][:], ALU.mult, ALU.add,
                              [B_cin[ch], B_c1, B_cacc[ch]], [B_cacc[ch]])
                for gi_ in range(2):
                    k.act(sq[gi_][:], sq[gi_][:], AF.Exp, [B_sq[gi_]], [B_sq[gi_]], scale=-0.5)
                for ch in range(3):
                    k.copy(CONVIN[:, ch, 0:3], CONVIN[:, ch, 512:515], [B_cin[ch]], [B_cin[ch]], eng="pool")
                    k.act(csil[ch][:], cacc[ch][:], AF.Exp, [B_cacc[ch]], [B_csil[ch]], scale=-1.0)
                    k.act(csil[ch][:], csil[ch][:], AF.Ln, [B_csil[ch]], [B_csil[ch]], bias=onesf[:, 0:1])
                    k.act(csil[ch][:], csil[ch][:], AF.Exp, [B_csil[ch]], [B_csil[ch]], scale=-1.0)
                    k.tt(csil[ch][:], csil[ch][:], cacc[ch][:], ALU.mult, [B_csil[ch], B_cacc[ch]], [B_csil[ch]])
                for gi_ in range(2):
                    si = 2 * par + gi_
                    k.stt(QS[si][:], psb[gi_][:, :], gain2[:, gi_:gi_ + 1], sq[gi_][:], ALU.mult, ALU.mult,
                          [B_ps[gi_], B_sq[gi_], B_c1], [B_QS[si]])
                    dstT, B_d = (QT, B_QT) if gi_ == 0 else (KT, B_KT)
                    k.dma("pool", dstT[0][0:64, t * 512:(t + 1) * 512], QS[si][0:64, :], [B_QS[si]], [B_d[0][t]],
                          dsem="qs%d_%d_a" % (par, gi_))
                    k.dma("pool", dstT[1][0:64, t * 512:(t + 1) * 512], QS[si][64:128, :], [B_QS[si]], [B_d[1][t]],
                          dsem="qs%d_%d_b" % (par, gi_))
                gs = t % 2
                for ch in range(2):
                    si = 2 * par + ch
                    k.act(sqb[si][:], csil[ch][:], AF.Square, [B_csil[ch]], [B_sqb[si]])
                k.copy(gst[gs][:, 2, :], csil[2][:], [B_csil[2]], [B_gst[gs]], eng="act")
                for ch in range(2):
                    si = 2 * par + ch
                    k.mm(psb[6][:, :], onesb[:, :], sqb[si][:], True, True, [B_sqb[si], B_c1], [B_ps[6]])
                    k.act(sq[4 + ch][:], psb[6][:, :], AF.Ln, [B_ps[6]], [B_sq[4 + ch]], scale=1.0, bias=epsc[:, 0:1])
                for ch in range(2):
                    k.act(sq[4 + ch][:], sq[4 + ch][:], AF.Exp, [B_sq[4 + ch]], [B_sq[4 + ch]], scale=-0.5)
                    k.tt(gst[gs][:, ch, :], csil[ch][:], sq[4 + ch][:], ALU.mult, [B_csil[ch], B_sq[4 + ch]], [B_gst[gs]])
                k.dma("pool", io["gdn_d"].rearrange("c p n -> p c n")[:, :, t * 512:(t + 1) * 512], gst[gs][:],
                      [B_gst[gs]], [ctx["B_gdn"]], dsem="gdn_w%d" % gs)

            def part4(t):
                hi = t % 2
                zs = t % 2
                for blk in range(4):
                    gb = 4 * t + blk
                    pb = 7 if blk % 2 == 0 else 5
                    for kc in range(8):
                        k.mm(psb[pb][:, 0:258], hT[hi][:, kc, blk * 128:(blk + 1) * 128], WAb[:, kc, 640:898], kc == 0, kc == 7,
                             [B_hT[hi], B_WAb], [B_ps[pb]])
                    k.copy(VA[:, gb, :], psb[pb][:, 0:128], [B_ps[pb]], [B_VA[t]], eng="act")
                    k.copy(zst[zs][:, blk, :], psb[pb][:, 128:256], [B_ps[pb]], [B_zst[zs]], eng="act")
                    k.copy(BA[:, gb, :], psb[pb][:, 256:258], [B_ps[pb]], [B_BA], eng="dve")
                k.dma("pool", io["z_d"][t * 512:(t + 1) * 512, :].rearrange("(b p) d -> p b d", p=128), zst[zs][:],
                      [B_zst[zs]], [ctx["B_zd"]], dsem="zd_w%d" % zs)

            part1(0)
            for t in range(NT):
                part2(t)
                if t + 1 < NT:
                    part1(t + 1)
                part4(t)
                part3(t)
            S.flush(st)

        if dbg.get("p1_lvl", 9) < 4:
            return
        with ExitStack() as s1b:
            biasT = sb(s1b, "biasT", [128, 64])
            dmask = sb(s1b, "dmask", [128, 4, 512])
            k.dma("sp", biasT[:], io["c_bias"], (), [B_c1], dsem="p1c0")
            k.dma("sp", dmask[:], io["c_dmask"].rearrange("p (d n) -> p d n", d=4), (), [B_c1], dsem="p1c1")
            Pt = [sb(s1b, "Pt%d" % i, [128, 512], BF16) for i in range(4)]
            B_Pt = [Buf("Pt%d" % i) for i in range(4)]
            S2 = [sb(s1b, "S2_%d" % i, [128, 512]) for i in range(2)]
            B_S2 = [Buf("S2_0"), Buf("S2_1")]
            Lacc = sb(s1b, "Lacc0", [128, 512])
            B_Lacc = Buf("Lacc0")
            Rinv = [sb(s1b, "Rinv%d" % i, [128, 512]) for i in range(2)]
            B_Rinv = [Buf("Rinv0"), Buf("Rinv1")]
            of = [sb(s1b, "of%d" % i, [128, 512]) for i in range(2)]
            B_of = [Buf("of0"), Buf("of1")]
            OAT = [sb(s1b, "OAT%d" % i, [128, 512], BF16) for i in range(2)]
            B_OAT = [Buf("OAT0"), Buf("OAT1")]
            stt_ = {"pcount": 0}

            def attention(t):
                nkb = 4 * t + 4
                steps = [(kb, m) for kb in range(nkb) for m in range(2)]
                LA = 2
                pbase = stt_["pcount"]
                stt_["pcount"] += len(steps)
                qs = slice(t * 512, (t + 1) * 512)

                def emit_s(i):
                    kb, m = steps[i]
                    d = kb - 4 * t
                    sbk = (pbase + i) % 3
                    if d < 0:
                        k.mm(psb[sbk][:, :], KT[m][0:67, kb * 128:(kb + 1) * 128], QT[m][0:67, qs], True, True,
                             [B_KT[m][kb // 4], B_KTaug, B_QT[m][t], B_QTaug], [B_ps[sbk]])
                    else:
                        k.mm(psb[sbk][:, :], KT[m][0:64, kb * 128:(kb + 1) * 128], QT[m][0:64, qs], True, True,
                             [B_KT[m][kb // 4], B_QT[m][t]], [B_ps[sbk]])

                for i in range(min(LA, len(steps))):
                    emit_s(i)
                for i, (kb, m) in enumerate(steps):
                    d = kb - 4 * t
                    sbk = (pbase + i) % 3
                    pi = (pbase + i) % 4
                    if d < 0:
                        n = 4 * t - kb
                        k.act(Pt[pi][:], psb[sbk][:, :], AF.Exp, [B_ps[sbk], B_c1], [B_Pt[pi]], bias=biasT[:, n:n + 1])
                    else:
                        s2i = (pbase + i) % 2
                        k.tt(S2[s2i][:], psb[sbk][:, :], dmask[:, d, :], ALU.add, [B_ps[sbk], B_c1], [B_S2[s2i]])
                        k.act(Pt[pi][:], S2[s2i][:], AF.Exp, [B_S2[s2i]], [B_Pt[pi]])
                    if i + LA < len(steps):
                        emit_s(i + LA)
                    k.mm(psb[3 + m][:, :], VA[:, kb, :], Pt[pi][:], kb == 0, kb == nkb - 1, [B_Pt[pi], B_VA[kb // 4]],
                         [B_ps[3 + m]])
                    k.mm(psb[5 + m][:, :], onesb[:, :], Pt[pi][:], kb == 0, kb == nkb - 1, [B_Pt[pi], B_c1], [B_ps[5 + m]])
                    yield

            def finalize(t):
                oi = t % 2
                for m in range(2):
                    k.act(Rinv[m][:], psb[5 + m][:, :], AF.Ln, [B_ps[5 + m]], [B_Rinv[m]])
                for m in range(2):
                    k.act(Rinv[m][:], Rinv[m][:], AF.Exp, [B_Rinv[m]], [B_Rinv[m]], scale=-1.0)
                k.tt(of[0][:], psb[3][:, :], Rinv[0][:], ALU.mult, [B_ps[3], B_Rinv[0]], [B_of[0]])
                k.tt(of[1][:], psb[4][:, :], Rinv[1][:], ALU.mult, [B_ps[4], B_Rinv[1]], [B_of[1]])
                yield
                k.stt(of[0][:], of[1][:], ctx["neglam"][:, 0:1], of[0][:], ALU.mult, ALU.add, [B_of[0], B_of[1], ctx["B_neglam"]], [B_of[0]])
                yield
                k.act(of[1][:], of[0][:], AF.Square, [B_of[0]], [B_of[1]])
                yield
                k.mm(psb[7][:, :], onesf[:, :], of[1][:], True, True, [B_of[1], B_const], [B_ps[7]])
                yield
                k.act(Rinv[0][:], psb[7][:, :], AF.Ln, [B_ps[7]], [B_Rinv[0]], scale=1.0 / 128, bias=epsc[:, 0:1])
                yield
                k.act(Rinv[0][:], Rinv[0][:], AF.Exp, [B_Rinv[0]], [B_Rinv[0]], scale=-0.5)
                yield
                k.stt(OAT[oi][:], of[0][:], onAc[:, 0:1], Rinv[0][:], ALU.mult, ALU.mult, [B_of[0], B_Rinv[0], B_c1], [B_OAT[oi]])
                k.dma("sp", io["exA_in"].ap()[t // 4, :, (t % 4) * 512:(t % 4 + 1) * 512], OAT[oi][:], [B_OAT[oi]],
                      [io["B_exA_in"][t // 4]], dsem="exin_a%d" % oi)
                if t % 4 == 3 and ctx.get("gatherA") is not None:
                    ctx["gatherA"](t // 4)
                yield

            fin = None
            for t in range(NT):
                for i_, _ in enumerate(attention(t)):
                    if fin is not None and i_ >= 1 and i_ % 2 == 1:
                        if next(fin, "done") == "done":
                            fin = None
                if fin is not None:
                    for _ in fin:
                        pass
                fin = finalize(t)
                next(fin)
            for _ in fin:
                pass
            S.flush(st)


def phase2(ctx, io):
    nc, S, k, st, sb = ctx["nc"], ctx["S"], ctx["k"], ctx["st"], ctx["sb"]
    psb, B_ps = ctx["psb"], ctx["B_ps"]
    dbg = ctx["dbg"]
    BA, B_BA = ctx["BA"], ctx["B_BA"]
    onesf, identf, identb, epsc, B_const = ctx["onesf"], ctx["identf"], ctx["identb"], ctx["epsc"], ctx["B_const"]
    NBLK = dbg.get("p2_blocks", 64)
    NG = NBLK // 4
    with ExitStack() as s2:
        gm = sb(s2, "gm", [128, 7, 128])
        TRI, SAMEC, SELA, SELB, MASKL, STRICT, MASKU = [gm[:, i, :] for i in range(7)]

        def bc_g(ap2):
            return ap2.unsqueeze(1).to_broadcast([128, 4, 128])

        def bc_c(ap_cols):
            return ap_cols.unsqueeze(2).to_broadcast([128, 4, 128])

        adt = sb(s2, "adt_sb", [128, 2])
        negA = sb(s2, "negA", [128, 1])
        onBt = sb(s2, "onBt", [128, 128])
        B_c2 = Buf("p2const")
        BETA = sb(s2, "BETA", [128, 64])
        Gt = sb(s2, "Gt", [128, 64])
        GC = sb(s2, "GC", [128, 64])
        GL = sb(s2, "GL", [128, 64])
        EG = sb(s2, "EG", [128, 64])
        EGL = sb(s2, "EGL", [128, 64])
        BG = sb(s2, "BG", [128, 64])
        NGC = sb(s2, "NGC", [128, 64])
        NBETA = sb(s2, "NBETA", [128, 64])
        GLB = sb(s2, "GLB", [128, 64, 2])
        B_bulk = Buf("p2bulk")
        Sst = sb(s2, "Sst", [128, 128])
        B_S = Buf("Sst")

        def mk(name, shape=(128, 4, 128), dt=F32, n=2):
            return [sb(s2, "%s%d" % (name, i), list(shape), dt) for i in range(n)], [Buf("%s%d" % (name, i)) for i in range(n)]
        qkv, B_qkv = mk("qkv", (128, 3, 512), BF16)
        KBG, B_KBG = mk("KBG")
        KDEC0, B_KDEC0 = mk("KDEC0")
        KDEC1, B_KDEC1 = mk("KDEC1")
        VB, B_VB = mk("VB")
        dG, B_dG = mk("dG", n=1)
        ER, B_ER = mk("ER", n=1)
        tD, B_tD = mk("tD", n=1)
        Dm, B_Dm = mk("Dm", n=1)
        tDT, B_tDT = mk("tDT", n=1)
        DT, B_DT = mk("DT", n=1)
        t3, B_t3 = mk("t3", n=1)
        Pa, B_Pa = mk("Pa", n=1)
        Pta, B_Pta = mk("Pta", n=1)
        Pb, B_Pb = mk("Pb", n=1)
        Ptb, B_Ptb = mk("Ptb", n=1)
        Tt, B_Tt = mk("Tt", n=1)
        QK0, B_QK0 = mk("QK0")
        QK1, B_QK1 = mk("QK1")
        QD0, B_QD0 = mk("QD0")
        QD1, B_QD1 = mk("QD1")
        WT0, B_WT0 = mk("WT0")
        WT1, B_WT1 = mk("WT1")
        U, B_U = mk("U")
        VN, B_VN = mk("VN", (128, 128), F32, 4)
        zt, B_zt = mk("zt")
        sz, B_sz = mk("sz", n=1)
        osb, B_osb = mk("osb2", n=1)
        ot, B_ot = mk("ot2", n=1)
        onb, B_onb = mk("onb2", (128, 4, 128), BF16, 1)
        ojk = sb(s2, "ojk2", [128, 128], BF16)
        B_ojk = Buf("ojk2")
        rr, B_rr = mk("rr2", (128, 12), F32, 1)
        OBT, B_OBT = mk("OBT", (128, 512), BF16)

        k.dma("sp", gm[:], io["c_gmask"].rearrange("p (a n) -> p a n", a=7), (), [B_c2], dsem="p2c0")
        k.dma("sp", adt[:], io["adt"].partition_broadcast(128), (), [B_c2], dsem="p2c1")
        k.dma("sp", onBt[:], io["onB"].partition_broadcast(128), (), [B_c2], dsem="p2c2")
        k.act(negA[:], adt[:, 0:1], AF.Exp, [B_c2], [B_c2])
        k.ts(negA[:], negA[:], -1.0, ALU.mult, [B_c2], [B_c2])
        for lst in (KDEC0, KDEC1, QK0, QK1, QD0, QD1, WT0, WT1):
            for tl in lst:
                k.memset(tl[:], 0.0, [B_c2], eng="dve")
        k.memset(Sst[:], 0.0, [B_S], eng="dve")
        RB, WB_ = [B_bulk, B_c2], [B_bulk]
        k.act(BETA[:], BA[:, :, 0], AF.Sigmoid, [B_BA], WB_)
        k.act(Gt[:], BA[:, :, 1], AF.Exp, [B_BA, B_c2], WB_, bias=adt[:, 1:2])
        k.act(Gt[:], Gt[:], AF.Ln, RB, WB_, bias=onesf[:, 0:1])
        k.ts(Gt[:], Gt[:], negA[:, 0:1], ALU.mult, RB, WB_)
        k.mm(psb[0][:, 0:64], TRI, Gt[:], True, True, RB, [B_ps[0]])
        k.mm(psb[1][:, 0:64], SAMEC, Gt[:], True, True, RB, [B_ps[1]])
        k.copy(GC[:], psb[0][:, 0:64], [B_ps[0]], WB_, eng="dve")
        k.copy(GL[:], psb[1][:, 0:64], [B_ps[1]], WB_, eng="dve")
        k.act(EG[:], GC[:], AF.Exp, RB, WB_)
        k.tt(EGL[:], GL[:], GC[:], ALU.subtract, RB, WB_)
        k.act(EGL[:], EGL[:], AF.Exp, RB, WB_)
        k.tt(BG[:], BETA[:], EG[:], ALU.mult, RB, WB_)
        k.ts(NGC[:], GC[:], -1.0, ALU.mult, RB, WB_)
        k.ts(NBETA[:], BETA[:], -1.0, ALU.mult, RB, WB_)
        k.mm(psb[2][:, 0:64], SELA, GL[:], True, True, RB, [B_ps[2]])
        k.mm(psb[3][:, 0:64], SELB, GL[:], True, True, RB, [B_ps[3]])
        k.act(GLB[:, :, 0], psb[2][:, 0:64], AF.Exp, [B_ps[2]], WB_)
        k.act(GLB[:, :, 1], psb[3][:, 0:64], AF.Exp, [B_ps[3]], WB_)

        gv = io["gdn_d"].rearrange("c p n -> p c n")

        def v4(t_):
            return t_.rearrange("p (g n) -> p g n", g=4)

        def pre(G):
            gp = G % 2
            n0 = 4 * G
            cs = slice(n0, n0 + 4)
            k.dma("sp", qkv[gp][:], gv[:, :, n0 * 128:(n0 + 4) * 128], [ctx["B_gdn"]], [B_qkv[gp]])
            k.dma("sp", zt[gp][:], io["z_d"][n0 * 128:(n0 + 4) * 128, :].rearrange("(g p) d -> p g d", p=128),
                  [ctx["B_zd"]], [B_zt[gp]])
            qT4 = v4(qkv[gp][:, 0, :])
            pv0 = psb[0][:, :].bitcast(BF16)
            for g in range(4):
                k.tr(pv0[:, g * 128:(g + 1) * 128], qkv[gp][:, 1, g * 128:(g + 1) * 128], identb[:], [B_qkv[gp], B_const], [B_ps[0]])
            for g in range(4):
                k.tr(pv0[:, 512 + g * 128:512 + (g + 1) * 128], qkv[gp][:, 2, g * 128:(g + 1) * 128], identb[:],
                     [B_qkv[gp], B_const], [B_ps[0]])
            k.tt(dG[0][:], bc_g(identf[:]), bc_c(GC[:, cs]), ALU.mult, [B_const, B_bulk], [B_dG[0]])
            yield
            k.mm(psb[1][:, :], onesf[:], dG[0][:].rearrange("p g n -> p (g n)"), True, True, [B_dG[0], B_const], [B_ps[1]])
            ktok = v4(pv0[:, 0:512])
            vtok = v4(pv0[:, 512:1024])
            k.tt(KBG[gp][:], ktok, bc_c(BG[:, cs]), ALU.mult, [B_ps[0], B_bulk], [B_KBG[gp]])
            k.tt(KDEC0[gp][0:64, :, :], ktok[0:64], bc_c(EGL[:, cs])[0:64], ALU.mult, [B_ps[0], B_bulk], [B_KDEC0[gp]])
            k.tt(KDEC1[gp][64:128, :, :], ktok[64:128], bc_c(EGL[:, cs])[64:128], ALU.mult, [B_ps[0], B_bulk], [B_KDEC1[gp]])
            k.tt(VB[gp][:], vtok, bc_c(BETA[:, cs]), ALU.mult, [B_ps[0], B_bulk], [B_VB[gp]])
            yield
            R4 = v4(psb[1][:, :])
            k.act(ER[0][:], R4, AF.Exp, [B_ps[1]], [B_ER[0]])
            k.stt(tD[0][:], R4, -1.0, bc_g(MASKL), ALU.mult, ALU.add, [B_ps[1], B_c2], [B_tD[0]])
            k.tt(tDT[0][:], R4, bc_g(MASKU), ALU.add, [B_ps[1], B_c2], [B_tDT[0]])
            for g in range(4):
                kTg = qkv[gp][:, 1, g * 128:(g + 1) * 128]
                k.mm(psb[2][:, g * 128:(g + 1) * 128], kTg, kTg, True, True, [B_qkv[gp]], [B_ps[2]])
            for g in range(4):
                kTg = qkv[gp][:, 1, g * 128:(g + 1) * 128]
                qTg = qkv[gp][:, 0, g * 128:(g + 1) * 128]
                k.mm(psb[3][:, g * 128:(g + 1) * 128], kTg, qTg, True, True, [B_qkv[gp]], [B_ps[3]])
            yield
            k.tt(QD0[gp][:, :, 0:64], qT4[:, :, 0:64], ER[0][:, :, 0:64], ALU.mult, [B_qkv[gp], B_ER[0]], [B_QD0[gp]])
            k.tt(QD1[gp][:, :, 64:128], qT4[:, :, 64:128], ER[0][:, :, 64:128], ALU.mult, [B_qkv[gp], B_ER[0]], [B_QD1[gp]])
            yield
            for g in range(4):
                k.act(Dm[0][:, g, :], tD[0][:, g, :], AF.Exp, [B_tD[0], B_bulk], [B_Dm[0]], bias=GC[:, n0 + g:n0 + g + 1])
            for g in range(4):
                k.act(DT[0][:, g, :], tDT[0][:, g, :], AF.Exp, [B_tDT[0], B_bulk], [B_DT[0]], bias=NGC[:, n0 + g:n0 + g + 1])
            yield
            k.tt(t3[0][:], v4(psb[2][:, :]), Dm[0][:], ALU.mult, [B_ps[2], B_Dm[0]], [B_t3[0]])
            k.tt(t3[0][:], t3[0][:], bc_c(NBETA[:, cs]), ALU.mult, [B_t3[0], B_bulk], [B_t3[0]])
            k.tt(Pa[0][:], t3[0][:], bc_g(STRICT), ALU.mult, [B_t3[0], B_c2], [B_Pa[0]])
            qk4 = v4(psb[3][:, :])
            k.tt(QK0[gp][:, :, 0:64], qk4[:, :, 0:64], DT[0][:, :, 0:64], ALU.mult, [B_ps[3], B_DT[0]], [B_QK0[gp]])
            k.tt(QK1[gp][:, :, 64:128], qk4[:, :, 64:128], DT[0][:, :, 64:128], ALU.mult, [B_ps[3], B_DT[0]], [B_QK1[gp]])
            yield
            for g in range(4):
                k.tr(psb[4][:, g * 128:(g + 1) * 128], Pa[0][:, g, :], identf[:], [B_Pa[0], B_const], [B_ps[4]])
            yield
            k.copy(Pta[0][:], v4(psb[4][:, :]), [B_ps[4]], [B_Pta[0]], eng="act")
            k.tt(Tt[0][:], Pta[0][:], bc_g(identf[:]), ALU.add, [B_Pta[0], B_const], [B_Tt[0]])
            yield
            P, BP, PT, BPT = Pa[0], B_Pa[0], Pta[0], B_Pta[0]
            Pn, BPn, PTn, BPTn = Pb[0], B_Pb[0], Ptb[0], B_Ptb[0]
            for lvl in range(1, 6):
                for g in range(4):
                    k.mm(psb[2][:, g * 128:(g + 1) * 128], PT[:, g, :], P[:, g, :], True, True, [BP, BPT], [B_ps[2]])
                if lvl < 5:
                    for g in range(4):
                        k.mm(psb[3][:, g * 128:(g + 1) * 128], P[:, g, :], PT[:, g, :], True, True, [BP, BPT], [B_ps[3]])
                yield
                k.copy(Pn[:], v4(psb[2][:, :]), [B_ps[2]], [BPn], eng="act")
                if lvl < 5:
                    k.copy(PTn[:], v4(psb[3][:, :]), [B_ps[3]], [BPTn], eng="dve")
                yield
                for g in range(4):
                    k.mm(psb[4][:, g * 128:(g + 1) * 128], Pn[:, g, :], Tt[0][:, g, :], True, True, [BPn, B_Tt[0]], [B_ps[4]])
                yield
                k.tt(Tt[0][:], Tt[0][:], v4(psb[4][:, :]), ALU.add, [B_Tt[0], B_ps[4]], [B_Tt[0]])
                yield
                P, BP, PT, BPT, Pn, BPn, PTn, BPTn = Pn, BPn, PTn, BPTn, P, BP, PT, BPT
            for g in range(4):
                k.mm(psb[2][:, g * 128:(g + 1) * 128], Tt[0][:, g, :], VB[gp][:, g, :], True, True, [B_Tt[0], B_VB[gp]], [B_ps[2]])
            for g in range(4):
                k.mm(psb[3][:, g * 128:(g + 1) * 128], KBG[gp][:, g, :], Tt[0][:, g, :], True, True, [B_Tt[0], B_KBG[gp]], [B_ps[3]])
            yield
            k.copy(U[gp][:], v4(psb[2][:, :]), [B_ps[2]], [B_U[gp]], eng="act")
            w4 = v4(psb[3][:, :])
            k.copy(WT0[gp][:, :, 0:64], w4[:, :, 0:64], [B_ps[3]], [B_WT0[gp]], eng="dve")
            k.copy(WT1[gp][:, :, 64:128], w4[:, :, 64:128], [B_ps[3]], [B_WT1[gp]], eng="dve")
            yield

        vn_state = {"i": 0}

        def scan(G):
            gp = G % 2
            n0 = 4 * G
            for g in range(4):
                n = n0 + g
                for c in range(2):
                    WTc, BWTc = (WT0[gp], B_WT0[gp]) if c == 0 else (WT1[gp], B_WT1[gp])
                    QDc, BQDc = (QD0[gp], B_QD0[gp]) if c == 0 else (QD1[gp], B_QD1[gp])
                    QKc, BQKc = (QK0[gp], B_QK0[gp]) if c == 0 else (QK1[gp], B_QK1[gp])
                    KDc, BKDc = (KDEC0[gp], B_KDEC0[gp]) if c == 0 else (KDEC1[gp], B_KDEC1[gp])
                    vi = vn_state["i"] % 4
                    vn_state["i"] += 1
                    p1 = psb[5][:, c * 128:(c + 1) * 128]
                    p2 = psb[5][:, 256 + c * 128:256 + (c + 1) * 128]
                    og = psb[6][:, g * 128:(g + 1) * 128]
                    k.mm(p1, WTc[:, g, :], Sst[:], True, True, [BWTc, B_S], [B_ps[5]])
                    k.mm(og, QDc[:, g, :], Sst[:], c == 0, False, [BQDc, B_S], [B_ps[6]], sgc=True)
                    yield
                    k.tt(VN[vi][:], U[gp][:, g, :], p1, ALU.subtract, [B_U[gp], B_ps[5]], [B_VN[vi]])
                    yield
                    k.mm(p2, KDc[:, g, :], VN[vi][:], True, True, [BKDc, B_VN[vi]], [B_ps[5]])
                    k.mm(og, QKc[:, g, :], VN[vi][:], False, c == 1, [BQKc, B_VN[vi]], [B_ps[6]], sgc=True)
                    yield
                    k.stt(Sst[:], Sst[:], GLB[:, n, c:c + 1], p2, ALU.mult, ALU.add, [B_S, B_bulk, B_ps[5]], [B_S])
                    yield
            r = rr[0]
            R_, W_ = [B_rr[0]], [B_rr[0]]
            k.act(osb[0][:], v4(psb[6][:, :]), AF.Identity, [B_ps[6]], [B_osb[0]], scale=128.0 ** -0.5)
            k.act(sz[0][:], zt[gp][:], AF.Exp, [B_zt[gp]], [B_sz[0]], scale=-1.0)
            k.act(sz[0][:], sz[0][:], AF.Ln, [B_sz[0]], [B_sz[0]], bias=onesf[:, 0:1])
            k.act(sz[0][:], sz[0][:], AF.Exp, [B_sz[0]], [B_sz[0]], scale=-1.0)
            k.tt(sz[0][:], sz[0][:], zt[gp][:], ALU.mult, [B_sz[0], B_zt[gp]], [B_sz[0]])
            yield
            for g in range(4):
                k.act(ojk[:], osb[0][:, g, :], AF.Square, [B_osb[0]], [B_ojk] + W_, accum_out=r[:, g:g + 1])
            k.act(r[:, 4:8], r[:, 0:4], AF.Ln, R_, W_, scale=1.0 / 128, bias=epsc[:, 0:1])
            k.act(r[:, 8:12], r[:, 4:8], AF.Exp, R_, W_, scale=-0.5)
            yield
            k.tt(ot[0][:], osb[0][:], bc_c(r[:, 8:12]), ALU.mult, [B_osb[0]] + R_, [B_ot[0]])
            k.tt(ot[0][:], ot[0][:], bc_g(onBt[:]), ALU.mult, [B_ot[0], B_c2], [B_ot[0]])
            k.tt(onb[0][:], ot[0][:], sz[0][:], ALU.mult, [B_ot[0], B_sz[0]], [B_onb[0]])
            yield
            pv7 = psb[7][:, :].bitcast(BF16)
            for g in range(4):
                k.tr(pv7[:, g * 128:(g + 1) * 128], onb[0][:, g, :], identb[:], [B_onb[0], B_const], [B_ps[7]])
            yield
            oi = G % 2
            k.copy(OBT[oi][:], pv7[:, 0:512], [B_ps[7]], [B_OBT[oi]], eng="act")
            k.dma("sp", io["exB_in"].ap()[G // 4, :, (G % 4) * 512:(G % 4 + 1) * 512], OBT[oi][:], [B_OBT[oi]],
                  [io["B_exB_in"][G // 4]], dsem="exin_b%d" % oi)
            if G % 4 == 3 and ctx.get("gatherB") is not None:
                ctx["gatherB"](G // 4)
            yield

        for _ in pre(0):
            pass
        for G in range(NG):
            nxt = pre(G + 1) if G + 1 < NG else None
            for i_, _ in enumerate(scan(G)):
                if nxt is not None and i_ % 1 == 0:
                    next(nxt, None)
            if nxt is not None:
                for _ in nxt:
                    pass
        S.flush(st)


def make_consts(h):
    slope = 2.0 ** (-2.0 * (h + 1))
    c = {}
    c["c_ident"] = np.eye(128, dtype=np.float32)
    jq = np.arange(512)
    c["c_aug_q"] = np.tile(np.stack([-slope * (jq // 256 * 256), -slope * (jq % 256), np.ones(512)]), (1, 16)).astype(np.float32)
    ik = np.arange(128)
    c["c_aug_k"] = np.tile(np.stack([np.ones(128), np.ones(128), slope * ik]), (1, 64)).astype(np.float32)
    c["c_bias"] = np.tile((-slope * 128.0 * np.arange(64))[None, :], (128, 1)).astype(np.float32)
    dm = np.zeros((128, 4, 512), np.float32)
    for d in range(4):
        kp = d * 128 + ik[:, None]
        qp = jq[None, :]
        ok = (kp // 64) <= (qp // 64)
        dm[:, d, :] = np.where(ok, -slope * np.abs(qp - kp), NEG)
    c["c_dmask"] = dm.reshape(128, 2048)
    a = np.arange(128)
    same = (a[:, None] // 64) == (a[None, :] // 64)
    tri = ((a[:, None] <= a[None, :]) & same).astype(np.float32)
    samec = same.astype(np.float32)
    sela = np.zeros((128, 128), np.float32)
    sela[0, :] = 1.0
    selb = np.zeros((128, 128), np.float32)
    selb[64, :] = 1.0
    maskl = np.where((a[:, None] >= a[None, :]) & same, 0.0, NEG).astype(np.float32)
    strictl = ((a[:, None] > a[None, :]) & same).astype(np.float32)
    c["c_gmask"] = np.concatenate([tri, samec, sela, selb, maskl, strictl, np.ascontiguousarray(maskl.T)], axis=1)
    return c


def prep_inputs(inp):
    f = lambda a: np.ascontiguousarray(np.asarray(a, dtype=np.float32))
    x = f(inp["x"])
    w_in = f(inp["w_in"])[0]
    shared = {
        "w_ada": f(inp["w_ada"])[0], "b_ada": f(inp["b_ada"])[0][None, :],
        "gain1": f(inp["norm1_gain"]), "gain2": f(inp["norm2_gain"]),
        "wG": np.ascontiguousarray(w_in[:, 3592:]),
        "qkg": np.ascontiguousarray(np.stack([f(inp["da_q_norm"])[0], f(inp["da_k_norm"])[0]], axis=1)),
        "lamv": np.ascontiguousarray(np.stack([f(inp["da_lambda_q1"])[0], f(inp["da_lambda_k1"])[0],
                                               f(inp["da_lambda_q2"])[0], f(inp["da_lambda_k2"])[0]], axis=1)),
        "onAc": np.ascontiguousarray(f(inp["da_out_norm"]).T), "onB": f(inp["gdn_out_norm"]),
        "w_ba": f(inp["w_branch_a"])[0], "w_bb": f(inp["w_branch_b"])[0], "w_out": f(inp["w_out"])[0],
        "w_rt": np.ascontiguousarray(np.concatenate([f(inp["w_group"])[0], f(inp["w_router"])[0]], axis=1)),
        "b_rt": np.ascontiguousarray(np.concatenate([f(inp["b_group"])[0], f(inp["b_router"])[0]])[None, :]),
        "w1": f(inp["w1"])[0], "w3": f(inp["w3"])[0], "w2": f(inp["w2"])[0],
    }
    conv = f(inp["gdn_conv"])[0]
    maps = []
    for c in range(8):
        b, h = c // 4, c % 4
        cols = np.concatenate([
            h * 128 + np.arange(128), 512 + h * 128 + np.arange(128),
            1536 + h * 128 + np.arange(128), 2048 + h * 128 + np.arange(128), 2560 + h * 128 + np.arange(128),
            1024 + h * 128 + np.arange(128), 3072 + h * 128 + np.arange(128),
            np.array([3584 + h, 3588 + h])])
        m = dict(shared)
        m["xb"] = x[b]
        m["xs"] = np.ascontiguousarray(x[b, h * NTOK_C:(h + 1) * NTOK_C])
        m["cT"] = np.ascontiguousarray(f(inp["c"])[b].reshape(8, 128).T)
        m["wA"] = np.ascontiguousarray(w_in[:, cols])
        m["convw"] = np.ascontiguousarray(
            np.stack([conv[:, t * 512 + h * 128: t * 512 + (h + 1) * 128] for t in range(3)], axis=0)
            .transpose(2, 0, 1).reshape(128, 12))
        m["adt"] = np.array([[f(inp["gdn_a_log"])[0, h], f(inp["gdn_dt_bias"])[0, h]]], np.float32)
        m.update(make_consts(h))
        maps.append(m)
    return maps


_PROG = None


def kernel(**inputs):
    global _PROG
    if _PROG is None:
        _PROG = build_program()
    nc, _ = _PROG
    maps = prep_inputs(inputs)
    res = run_bass_kernel_spmd(nc, maps, core_ids=list(range(8)))
    out = np.empty((2, SEQ, D), np.float32)
    for c in range(8):
        b, j = c // 4, c % 4
        out[b, j * NTOK_C:(j + 1) * NTOK_C] = res.results[c]["out"]
    return out
```
